# Optimizing a Trainium2 kernel written in Bass

```python
import jax, jax.numpy as jnp
from jax import lax
import numpy as np

D_MODEL = 1024
BATCH = 8
SEQ = 4096
DEPTH = 2

HEAD_DIM = 64
ATT_HEADS = 8
ATT_WIDTH = ATT_HEADS * HEAD_DIM
LRU_BLOCKS = 4
LRU_WIDTH = LRU_BLOCKS * HEAD_DIM
RWKV_HEADS = 4
RWKV_WIDTH = RWKV_HEADS * HEAD_DIM
MIX_WIDTH = ATT_WIDTH + LRU_WIDTH + RWKV_WIDTH
ROPE_DIMS = HEAD_DIM // 4
ROPE_THETA = 500000.0
MOBA_BLOCK = 256
MOBA_TOPK = 3
MOBA_Q_CHUNK = 32
CONV_WIDTH = 4
LRU_C = 8.0
RWKV_DECAY_RANK = 64
RWKV_A_RANK = 64
RWKV_GATE_RANK = 128
RWKV_LN_EPS = 64e-5
RWKV_PROJ = 3 * RWKV_WIDTH + RWKV_DECAY_RANK + RWKV_A_RANK + RWKV_GATE_RANK
IN_WIDTH = 3 * ATT_WIDTH + 2 * LRU_WIDTH + RWKV_PROJ
IN_SPLITS = [ATT_WIDTH, 2 * ATT_WIDTH, 3 * ATT_WIDTH,
             3 * ATT_WIDTH + LRU_WIDTH, 3 * ATT_WIDTH + 2 * LRU_WIDTH]
RWKV_SPLITS = [RWKV_WIDTH, 2 * RWKV_WIDTH, 3 * RWKV_WIDTH,
               3 * RWKV_WIDTH + RWKV_DECAY_RANK, 3 * RWKV_WIDTH + RWKV_DECAY_RANK + RWKV_A_RANK]
D_FF = 2816
N_EXPERTS = 8
TOP_K = 2
D_EXPERT = 3584
MOE_BLOCK = 512
LN_EPS = 1e-5
DEEPNORM_ALPHA = (2 * DEPTH) ** 0.25
DEEPNORM_BETA = (8 * DEPTH) ** -0.25
N_DENSE = (DEPTH + 1) // 2
N_MOE = DEPTH // 2
NEG_INF = -1e30

kernel_name = "hybrid_moba_rglru_rwkv7_deepnorm"


def layer_norm(x, g, b):
    xf = x.astype(jnp.float32)
    m = xf.mean(-1, keepdims=True)
    var = jnp.square(xf - m).mean(-1, keepdims=True)
    return ((xf - m) * lax.rsqrt(var + LN_EPS) * g + b).astype(x.dtype)


def partial_rotary(x, cos, sin):
    half = ROPE_DIMS // 2
    x1 = x[..., :half]
    x2 = x[..., half:ROPE_DIMS]
    return jnp.concatenate([x1 * cos - x2 * sin, x2 * cos + x1 * sin, x[..., ROPE_DIMS:]], axis=-1)


def moba_attention(q, k, v):
    B, H, S, Dh = q.shape
    nb = max(-(-S // MOBA_BLOCK), MOBA_TOPK)
    pad = nb * MOBA_BLOCK - S
    kp = jnp.pad(k, ((0, 0), (0, 0), (0, pad), (0, 0)))
    vp = jnp.pad(v, ((0, 0), (0, 0), (0, pad), (0, 0)))
    kb = kp.reshape(B, H, nb, MOBA_BLOCK, Dh)
    vb = vp.reshape(B, H, nb, MOBA_BLOCK, Dh)
    k_mean = kb.astype(jnp.float32).mean(axis=3).astype(q.dtype)
    scale = Dh ** -0.5
    bi = jnp.arange(B)[:, None, None, None]
    hi = jnp.arange(H)[None, :, None, None]
    n_sel = MOBA_TOPK * MOBA_BLOCK

    def chunk(c):
        start = c * MOBA_Q_CHUNK
        qblk = start // MOBA_BLOCK
        qc = lax.dynamic_slice_in_dim(q, start, MOBA_Q_CHUNK, axis=2)
        gate = jnp.einsum('bhqd,bhnd->bhqn', qc, k_mean).astype(jnp.float32)
        gate = jnp.where(jnp.arange(nb) < qblk, gate, -jnp.inf)
        _, sel = lax.top_k(gate, MOBA_TOPK)
        slot_ok = jnp.arange(MOBA_TOPK) < qblk
        k_sel = kb[bi, hi, sel]
        v_sel = vb[bi, hi, sel]
        s_sel = jnp.einsum('bhqd,bhqkld->bhqkl', qc, k_sel).astype(jnp.float32) * scale
        s_sel = jnp.where(slot_ok[:, None], s_sel, NEG_INF)
        own = qblk * MOBA_BLOCK
        k_own = lax.dynamic_slice_in_dim(kp, own, MOBA_BLOCK, axis=2)
        v_own = lax.dynamic_slice_in_dim(vp, own, MOBA_BLOCK, axis=2)
        s_own = jnp.einsum('bhqd,bhld->bhql', qc, k_own).astype(jnp.float32) * scale
        causal = (own + jnp.arange(MOBA_BLOCK))[None, :] <= (start + jnp.arange(MOBA_Q_CHUNK))[:, None]
        s_own = jnp.where(causal, s_own, NEG_INF)
        logits = jnp.concatenate([s_sel.reshape(B, H, MOBA_Q_CHUNK, n_sel), s_own], axis=-1)
        p = jax.nn.softmax(logits, axis=-1).astype(v.dtype)
        p_sel = p[..., :n_sel].reshape(B, H, MOBA_Q_CHUNK, MOBA_TOPK, MOBA_BLOCK)
        return (jnp.einsum('bhqkl,bhqkld->bhqd', p_sel, v_sel)
                + jnp.einsum('bhql,bhld->bhqd', p[..., n_sel:], v_own))

    out = lax.map(chunk, jnp.arange(S // MOBA_Q_CHUNK))
    return out.transpose(1, 2, 0, 3, 4).reshape(B, H, S, Dh)


def rglru_mixer(xb, gb, conv_w, conv_b, ga_w, ga_b, gx_w, gx_b, lam):
    B, S, _ = xb.shape
    xp = jnp.pad(xb, ((0, 0), (CONV_WIDTH - 1, 0), (0, 0)))
    xc = conv_b
    for j in range(CONV_WIDTH):
        xc = xc + xp[:, j:j + S] * conv_w[j]
    xh = xc.reshape(B, S, LRU_BLOCKS, HEAD_DIM)
    r = jax.nn.sigmoid(jnp.einsum('bsnd,nde->bsne', xh, ga_w).reshape(B, S, LRU_WIDTH) + ga_b)
    i = jax.nn.sigmoid(jnp.einsum('bsnd,nde->bsne', xh, gx_w).reshape(B, S, LRU_WIDTH) + gx_b)
    log_a = (-LRU_C * r * jax.nn.softplus(-lam)).astype(jnp.float32)
    a = jnp.exp(log_a)
    u = jnp.sqrt(-jnp.expm1(2.0 * log_a)) * (i * xc).astype(jnp.float32)

    def combine(lhs, rhs):
        a1, b1 = lhs
        a2, b2 = rhs
        return a1 * a2, a2 * b1 + b2

    _, h = lax.associative_scan(combine, (a, u), axis=1)
    return h.astype(xb.dtype) * jax.nn.gelu(gb)


def wkv7_scan(r, w, k, v, a, b):
    B, S, H, N = r.shape

    def step(state, inp):
        r_t, w_t, k_t, v_t, a_t, b_t = inp
        sa = jnp.einsum('bhvk,bhk->bhv', state, a_t)
        state = (state * w_t[:, :, None, :] + sa[..., None] * b_t[:, :, None, :]
                 + v_t[..., None] * k_t[:, :, None, :])
        return state, jnp.einsum('bhvk,bhk->bhv', state, r_t)

    xs = tuple(t.transpose(1, 0, 2, 3) for t in (r, w, k, v, a, b))
    _, ys = lax.scan(step, jnp.zeros((B, H, N, N), jnp.float32), xs)
    return ys.transpose(1, 0, 2, 3)


def rwkv7_mixer(p, mu, w0, w_up, a0, a_up, g_up, k_k, k_a, r_k, lnx_g, lnx_b):
    B, S, _ = p.shape
    pf = p.astype(jnp.float32)
    p_prev = jnp.pad(pf, ((0, 0), (1, 0), (0, 0)))[:, :S]
    pf = pf + (p_prev - pf) * mu
    r, k, v, xw, xa, xg = jnp.split(pf, RWKV_SPLITS, axis=-1)
    w = -jax.nn.softplus(-(w0 + jnp.tanh(xw) @ w_up)) - 0.5
    decay = jnp.exp(-jnp.exp(w))
    a = jax.nn.sigmoid(a0 + xa @ a_up)
    g = jax.nn.sigmoid(xg) @ g_up
    hs = lambda t: t.reshape(B, S, RWKV_HEADS, HEAD_DIM)
    kk = hs(k * k_k)
    kk = kk / jnp.maximum(jnp.sqrt(jnp.sum(kk * kk, axis=-1, keepdims=True)), 1e-12)
    k = k * (1.0 + (a - 1.0) * k_a)
    rh, kh, vh, ah = hs(r), hs(k), hs(v), hs(a)
    y = wkv7_scan(rh, hs(decay), kh, vh, -kk, kk * ah)
    m = y.mean(-1, keepdims=True)
    var = jnp.square(y - m).mean(-1, keepdims=True)
    y = ((y - m) * lax.rsqrt(var + RWKV_LN_EPS)).reshape(B, S, RWKV_WIDTH) * lnx_g + lnx_b
    bonus = jnp.sum(rh * kh * r_k, axis=-1, keepdims=True) * vh
    y = y + bonus.reshape(B, S, RWKV_WIDTH)
    return (y * g).astype(p.dtype)


def hybrid_mixer(x, cos, sin, w_in, conv_w, conv_b, ga_w, ga_b, gx_w, gx_b, lam,
                 mu, w0, w_up, a0, a_up, g_up, k_k, k_a, r_k, lnx_g, lnx_b, w_out):
    B, S, _ = x.shape
    proj = x @ w_in
    q, k, v, lru_x, lru_g, rwkv_p = jnp.split(proj, IN_SPLITS, axis=-1)
    heads = lambda t: t.reshape(B, S, ATT_HEADS, HEAD_DIM)
    q = partial_rotary(heads(q), cos, sin).transpose(0, 2, 1, 3)
    k = partial_rotary(heads(k), cos, sin).transpose(0, 2, 1, 3)
    v = heads(v).transpose(0, 2, 1, 3)
    att = moba_attention(q, k, v).transpose(0, 2, 1, 3).reshape(B, S, ATT_WIDTH)
    lru = rglru_mixer(lru_x, lru_g, conv_w, conv_b, ga_w, ga_b, gx_w, gx_b, lam)
    rwk = rwkv7_mixer(rwkv_p, mu, w0, w_up, a0, a_up, g_up, k_k, k_a, r_k, lnx_g, lnx_b)
    return jnp.concatenate([att, lru, rwk], axis=-1) @ w_out


def swiglu(x, w_gate, w_up, w_down):
    return (jax.nn.silu(x @ w_gate) * (x @ w_up)) @ w_down


def moe_swiglu(x, w_router, w_gate, w_up, w_down):
    T, D = x.shape
    logits = (x @ w_router).astype(jnp.float32)
    top_val, top_idx = lax.top_k(logits, TOP_K)
    gates = jax.nn.softmax(top_val, axis=-1)
    expert_of = top_idx.reshape(-1)
    token_of = jnp.repeat(jnp.arange(T, dtype=jnp.int32), TOP_K)
    gate_of = gates.reshape(-1)
    onehot = jax.nn.one_hot(expert_of, N_EXPERTS, dtype=jnp.int32)
    rank = jnp.sum(jnp.cumsum(onehot, axis=0) * onehot, axis=-1) - 1
    counts = onehot.sum(0)
    padded = (counts + MOE_BLOCK - 1) // MOE_BLOCK * MOE_BLOCK
    pad_end = jnp.cumsum(padded)
    dest = pad_end[expert_of] - padded[expert_of] + rank
    n_rows = -(-(T * TOP_K) // MOE_BLOCK) * MOE_BLOCK + N_EXPERTS * MOE_BLOCK
    n_blocks = n_rows // MOE_BLOCK
    row_tok = jnp.zeros((n_rows,), jnp.int32).at[dest].set(token_of)
    row_gate = jnp.zeros((n_rows,), jnp.float32).at[dest].set(gate_of)
    blk_start = jnp.arange(n_blocks, dtype=jnp.int32) * MOE_BLOCK
    blk_expert = jnp.minimum(jnp.searchsorted(pad_end, blk_start, side='right'), N_EXPERTS - 1)
    xs = x[row_tok].reshape(n_blocks, MOE_BLOCK, D)

    def expert_block(args):
        xb, e = args
        return (jax.nn.silu(xb @ w_gate[e]) * (xb @ w_up[e])) @ w_down[e]

    ys = lax.map(expert_block, (xs, blk_expert)).reshape(n_rows, D)
    ys = ys * row_gate[:, None].astype(ys.dtype)
    return jnp.zeros_like(x).at[row_tok].add(ys)


def setup_inputs(seed: int = 0) -> dict:
    key = jax.random.key(seed)
    ks = iter(jax.random.split(key, 48))
    f32 = jnp.float32
    L = DEPTH

    def nrm(shape, scale):
        return jax.random.normal(next(ks), shape, f32) * scale

    x = nrm((BATCH, SEQ, D_MODEL), 1.0)
    offset = jax.random.randint(next(ks), (BATCH, 1), 0, 1024, dtype=jnp.int32)
    positions = offset + jnp.arange(SEQ, dtype=jnp.int32)[None, :]
    u = jax.random.uniform(next(ks), (L, LRU_WIDTH), f32, 0.9, 0.999)
    return {
        "x": x,
        "positions": positions,
        "w_in": nrm((L, D_MODEL, IN_WIDTH), D_MODEL ** -0.5),
        "lru_conv_w": nrm((L, CONV_WIDTH, LRU_WIDTH), CONV_WIDTH ** -0.5),
        "lru_conv_b": nrm((L, LRU_WIDTH), 0.01),
        "lru_ga_w": nrm((L, LRU_BLOCKS, HEAD_DIM, HEAD_DIM), HEAD_DIM ** -0.5),
        "lru_ga_b": nrm((L, LRU_WIDTH), 0.01),
        "lru_gx_w": nrm((L, LRU_BLOCKS, HEAD_DIM, HEAD_DIM), HEAD_DIM ** -0.5),
        "lru_gx_b": nrm((L, LRU_WIDTH), 0.01),
        "lru_lambda": jnp.log(u) - jnp.log1p(-u),
        "rwkv_mu": jax.random.uniform(next(ks), (L, RWKV_PROJ), f32, 0.0, 1.0),
        "rwkv_w0": jnp.linspace(-6.0, -1.0, RWKV_WIDTH, dtype=f32)[None, :] + nrm((L, RWKV_WIDTH), 0.1),
        "rwkv_w_up": nrm((L, RWKV_DECAY_RANK, RWKV_WIDTH), 0.1),
        "rwkv_a0": nrm((L, RWKV_WIDTH), 0.1),
        "rwkv_a_up": nrm((L, RWKV_A_RANK, RWKV_WIDTH), 0.5 * RWKV_A_RANK ** -0.5),
        "rwkv_g_up": nrm((L, RWKV_GATE_RANK, RWKV_WIDTH), RWKV_GATE_RANK ** -0.5),
        "rwkv_k_k": 0.85 + nrm((L, RWKV_WIDTH), 0.02),
        "rwkv_k_a": 1.0 + nrm((L, RWKV_WIDTH), 0.02),
        "rwkv_r_k": nrm((L, RWKV_HEADS, HEAD_DIM), 0.1),
        "rwkv_lnx_g": 1.0 + nrm((L, RWKV_WIDTH), 0.02),
        "rwkv_lnx_b": nrm((L, RWKV_WIDTH), 0.01),
        "w_out": nrm((L, MIX_WIDTH, D_MODEL), DEEPNORM_BETA * MIX_WIDTH ** -0.5),
        "ln1_g": 1.0 + nrm((L, D_MODEL), 0.02),
        "ln1_b": nrm((L, D_MODEL), 0.01),
        "ffn_w_gate": nrm((N_DENSE, D_MODEL, D_FF), D_MODEL ** -0.5),
        "ffn_w_up": nrm((N_DENSE, D_MODEL, D_FF), D_MODEL ** -0.5),
        "ffn_w_down": nrm((N_DENSE, D_FF, D_MODEL), DEEPNORM_BETA * D_FF ** -0.5),
        "moe_router": nrm((N_MOE, D_MODEL, N_EXPERTS), D_MODEL ** -0.5),
        "moe_w_gate": nrm((N_MOE, N_EXPERTS, D_MODEL, D_EXPERT), D_MODEL ** -0.5),
        "moe_w_up": nrm((N_MOE, N_EXPERTS, D_MODEL, D_EXPERT), D_MODEL ** -0.5),
        "moe_w_down": nrm((N_MOE, N_EXPERTS, D_EXPERT, D_MODEL), DEEPNORM_BETA * D_EXPERT ** -0.5),
        "ln2_g": 1.0 + nrm((L, D_MODEL), 0.02),
        "ln2_b": nrm((L, D_MODEL), 0.01),
    }


def reference(x, positions, w_in, lru_conv_w, lru_conv_b, lru_ga_w, lru_ga_b, lru_gx_w, lru_gx_b, lru_lambda,
              rwkv_mu, rwkv_w0, rwkv_w_up, rwkv_a0, rwkv_a_up, rwkv_g_up, rwkv_k_k, rwkv_k_a, rwkv_r_k,
              rwkv_lnx_g, rwkv_lnx_b, w_out, ln1_g, ln1_b, ffn_w_gate, ffn_w_up, ffn_w_down,
              moe_router, moe_w_gate, moe_w_up, moe_w_down, ln2_g, ln2_b):
    B, S, D = x.shape
    inv_freq = ROPE_THETA ** (-jnp.arange(0, ROPE_DIMS, 2, dtype=jnp.float32) / ROPE_DIMS)
    ang = positions.astype(jnp.float32)[..., None] * inv_freq
    cos = jnp.cos(ang)[:, :, None, :].astype(x.dtype)
    sin = jnp.sin(ang)[:, :, None, :].astype(x.dtype)
    for l in range(DEPTH):
        h = hybrid_mixer(x, cos, sin, w_in[l], lru_conv_w[l], lru_conv_b[l], lru_ga_w[l], lru_ga_b[l],
                         lru_gx_w[l], lru_gx_b[l], lru_lambda[l], rwkv_mu[l], rwkv_w0[l], rwkv_w_up[l],
                         rwkv_a0[l], rwkv_a_up[l], rwkv_g_up[l], rwkv_k_k[l], rwkv_k_a[l], rwkv_r_k[l],
                         rwkv_lnx_g[l], rwkv_lnx_b[l], w_out[l])
        x = layer_norm(DEEPNORM_ALPHA * x + h, ln1_g[l], ln1_b[l])
        if l % 2 == 0:
            f = swiglu(x, ffn_w_gate[l // 2], ffn_w_up[l // 2], ffn_w_down[l // 2])
        else:
            f = moe_swiglu(x.reshape(B * S, D), moe_router[l // 2], moe_w_gate[l // 2],
                           moe_w_up[l // 2], moe_w_down[l // 2]).reshape(B, S, D)
        x = layer_norm(DEEPNORM_ALPHA * x + f, ln2_g[l], ln2_b[l])
    return x
```

```python
import math
from contextlib import ExitStack

import ml_dtypes
import numpy as np

import concourse.bass as bass
import concourse.mybir as mybir
from concourse.bass_utils import run_bass_kernel_spmd

F32, BF16, I32 = mybir.dt.float32, mybir.dt.bfloat16, mybir.dt.int32
ALU = mybir.AluOpType
AF = mybir.ActivationFunctionType
AX = mybir.AxisListType

S_ = 4096
D_ = 1024
NT = 32
NST = 8
ALPHA = 4 ** 0.25
NEG = -30000.0
RWKV_STAGE = 4
DENSE_MOE = False
PI = math.pi


class Sch:
    CE = ("pe", "act", "dve", "pool")

    def __init__(self, nc, es):
        self.nc = nc
        self.eng = {"pe": nc.tensor, "act": nc.scalar, "dve": nc.vector, "pool": nc.gpsimd, "sp": nc.sync}
        self.sem = {e: es.enter_context(nc.semaphore("s_" + e)) for e in self.CE}
        self.cnt = {e: 0 for e in self.CE}
        self.NDS = 48
        self.dsem = [es.enter_context(nc.semaphore(f"d{i}")) for i in range(self.NDS)]
        self.dval = [0] * self.NDS
        self.dnext = 0
        self.known = {e: {} for e in self.eng}
        self.lastw = {}
        self.readers = {}
        self.pend = {e: (set(), set()) for e in self.CE}

    def _wait(self, e, tok):
        if tok is None:
            return
        kind, who, val = tok
        if kind == "c" and who == e:
            if e == "pe":
                return
            if e != "pool" and self.cnt[e] - val >= 2:
                return
        k = (kind, who)
        if self.known[e].get(k, 0) >= val:
            return
        self.known[e][k] = val
        s = self.sem[who] if kind == "c" else self.dsem[who]
        self.eng[e].wait_ge(s, val)

    def deps(self, e, reads, writes):
        for k in reads:
            self._wait(e, self.lastw.get(k))
        for k in writes:
            self._wait(e, self.lastw.get(k))
            rd = self.readers.get(k)
            if rd:
                for (kind, who), val in list(rd.items()):
                    self._wait(e, (kind, who, val))

    def commit(self, tok, reads, writes):
        kk = (tok[0], tok[1])
        for k in reads:
            d = self.readers.setdefault(k, {})
            if d.get(kk, 0) < tok[2]:
                d[kk] = tok[2]
        for k in writes:
            self.lastw[k] = tok
            self.readers[k] = {}

    def op(self, e, fn, r=(), w=(), sig=True):
        self.deps(e, r, w)
        inst = fn(self.eng[e])
        pr, pw = self.pend[e]
        pr.update(r)
        pw.update(w)
        if sig:
            self.cnt[e] += 1
            inst.then_inc(self.sem[e], 1)
            self.commit(("c", e, self.cnt[e]), pr, pw)
            self.pend[e] = (set(), set())

    def dma(self, q, out, in_, r=(), w=(), **kw):
        i = self.dnext
        self.dnext = (self.dnext + 1) % self.NDS
        if self.dval[i] > 0:
            self._wait(q, ("d", i, self.dval[i]))
        self.deps(q, r, w)
        inst = self.eng[q].dma_start(out=out, in_=in_, **kw)
        self.dval[i] += 16
        inst.then_inc(self.dsem[i], 16)
        self.commit(("d", i, self.dval[i]), r, w)

    def barrier(self):
        for e in self.eng:
            for f in self.CE:
                if self.cnt[f] > 0:
                    self._wait(e, ("c", f, self.cnt[f]) if f != e or e == "pool" else None)
            for i in range(self.NDS):
                if self.dval[i] > 0:
                    self._wait(e, ("d", i, self.dval[i]))
        self.lastw = {}
        self.readers = {}

    def mm(self, out, lhsT, rhs, start=True, stop=True, r=(), w=(), sig=True):
        self.op("pe", lambda e: e.matmul(out, lhsT=lhsT, rhs=rhs, start=start, stop=stop,
                                         skip_group_check=True), r, w, sig)

    def tr(self, out, in_, ident, r=(), w=(), sig=True):
        self.op("pe", lambda e: e.transpose(out, in_, ident), r, w, sig)

    def act(self, out, in_, func, r=(), w=(), eng="act", **kw):
        self.op(eng, lambda e: e.activation(out=out, in_=in_, func=func, **kw), r, w)

    def copy(self, eng, out, in_, r=(), w=()):
        if eng == "act":
            self.op(eng, lambda e: e.activation(out=out, in_=in_, func=AF.Copy), r, w)
        else:
            self.op(eng, lambda e: e.tensor_copy(out=out, in_=in_), r, w)

    def tt(self, eng, out, in0, in1, op, r=(), w=()):
        self.op(eng, lambda e: e.tensor_tensor(out=out, in0=in0, in1=in1, op=op), r, w)

    def ts(self, eng, out, in0, s1, s2, op0, op1=None, r=(), w=()):
        if op1 is None:
            self.op(eng, lambda e: e.tensor_scalar(out=out, in0=in0, scalar1=s1, scalar2=None, op0=op0), r, w)
        else:
            self.op(eng, lambda e: e.tensor_scalar(out=out, in0=in0, scalar1=s1, scalar2=s2, op0=op0, op1=op1), r, w)

    def stt(self, eng, out, in0, scalar, in1, op0, op1, r=(), w=()):
        self.op(eng, lambda e: e.scalar_tensor_tensor(out=out, in0=in0, scalar=scalar, in1=in1, op0=op0, op1=op1),
                r, w)

    def memset(self, eng, ap, val, w=()):
        self.op(eng, lambda e: e.memset(ap, val), (), w)


def make_consts():
    bf = ml_dtypes.bfloat16
    c = {}
    c["ident_bf"] = np.eye(128, dtype=np.float32).astype(bf)
    c["ident_f"] = np.eye(128, dtype=np.float32)
    blk = np.zeros((16, S_), np.float32)
    for n in range(16):
        blk[n, n * 256:(n + 1) * 256] = 1.0
    c["blkind"] = blk.astype(bf)
    k = np.arange(128)[:, None, None]
    d = np.arange(4)[None, :, None]
    q = np.arange(512)[None, None, :]
    c["causal"] = np.where(q >= d * 128 + k, 0.0, NEG).astype(np.float32).astype(bf)
    g1 = np.zeros((128, NT, 16), np.float32)
    g2 = np.zeros((128, NT, 16), np.float32)
    for i in range(NT):
        qb = i // 2
        for n in range(16):
            if n >= qb:
                g1[:, i, n] = -1e30
            if n == qb:
                g2[:, i, n] = 1e30
            elif n > qb:
                g2[:, i, n] = -2e30
    c["gmask1"] = g1.reshape(128, NT * 16)
    c["gmask2"] = g2.reshape(128, NT * 16)
    rm = np.ones((128, 512), np.float32)
    rm[:, ::64] = 0.0
    c["rmask"] = rm
    su = np.triu(np.ones((64, 64), np.float32), 1)
    ui = np.triu(np.ones((64, 64), np.float32), 0)
    c["mtmask"] = np.concatenate([su, ui, su, ui], axis=1)
    c["slmask"] = np.tril(np.ones((64, 64), np.float32), -1)
    s65 = np.zeros((65, 64), np.float32)
    s65[64, :] = 1.0
    c["sel65"] = s65
    bo = np.zeros((128, 128), np.float32)
    bo[:64, :64] = 1.0
    bo[64:, 64:] = 1.0
    c["bones"] = bo
    hs = np.zeros((128, 2), np.float32)
    hs[:64, 0] = 1.0
    hs[64:, 1] = 1.0
    c["hsel"] = hs
    inv = (500000.0 ** (-np.arange(0, 16, 2, dtype=np.float32) / 16)).astype(np.float32)
    c["invf"] = np.tile(inv[None, :], (128, 1)).astype(np.float32)
    c["ltri_bf"] = np.triu(np.ones((128, 128), np.float32), 1).astype(bf)
    ec = np.zeros((128, 2, 8), np.float32)
    ec[:, 0, :] = np.arange(8, dtype=np.float32) * 1280.0
    ec[:, 1, :] = (np.arange(8, dtype=np.float32) + 1.0) * 1280.0
    c["ecap"] = ec
    return c


CONST_SPECS = {
    "ident_bf": ([128, 128], BF16), "ident_f": ([128, 128], F32), "blkind": ([16, S_], BF16),
    "causal": ([128, 4, 512], BF16), "gmask1": ([128, NT * 16], F32), "gmask2": ([128, NT * 16], F32),
    "rmask": ([128, 512], F32), "mtmask": ([64, 256], F32), "slmask": ([64, 64], F32),
    "sel65": ([65, 64], F32), "bones": ([128, 128], F32), "hsel": ([128, 2], F32), "invf": ([128, 8], F32), "ltri_bf": ([128, 128], BF16), "ecap": ([128, 2, 8], F32),
}

WEIGHT_SPECS = {
    "w_in": [2, 1024, 3072], "lru_conv_w": [2, 4, 256], "lru_conv_b": [2, 256], "lru_ga_w": [2, 4, 64, 64],
    "lru_ga_b": [2, 256], "lru_gx_w": [2, 4, 64, 64], "lru_gx_b": [2, 256], "lru_lambda": [2, 256],
    "rwkv_mu": [2, 1024], "rwkv_w0": [2, 256], "rwkv_w_up": [2, 64, 256], "rwkv_a0": [2, 256],
    "rwkv_a_up": [2, 64, 256], "rwkv_g_up": [2, 128, 256], "rwkv_k_k": [2, 256], "rwkv_k_a": [2, 256],
    "rwkv_r_k": [2, 4, 64], "rwkv_lnx_g": [2, 256], "rwkv_lnx_b": [2, 256], "w_out": [2, 1024, 1024],
    "ln1_g": [2, 1024], "ln1_b": [2, 1024], "ffn_w_gate": [1, 1024, 2816], "ffn_w_up": [1, 1024, 2816],
    "ffn_w_down": [1, 2816, 1024], "moe_router": [1, 1024, 8], "moe_w_gate": [1, 8, 1024, 3584],
    "moe_w_up": [1, 8, 1024, 3584], "moe_w_down": [1, 8, 3584, 1024], "ln2_g": [2, 1024], "ln2_b": [2, 1024],
}


_PH = [0]


def rot(lst, state, key):
    i = state.get(key, 0)
    state[key] = i + 1
    return i % len(lst), lst[i % len(lst)]


def phase_inproj(sc, nc, dr, l, xsrc):
    _PH[0] += 1
    with ExitStack() as ps:
        def sb(name, shape, dt):
            return ps.enter_context(nc.sbuf_tensor(f"u{_PH[0]}_p1_" + name, shape, dt))

        def pm(name, shape, dt):
            return ps.enter_context(nc.psum_tensor(f"u{_PH[0]}_p1_" + name, shape, dt))

        st = {}
        win = sb("win", [128, 8, 3072], BF16)
        for kc in range(8):
            sc.dma("pool", win[:, kc, :], dr["w_in"][l, kc * 128:(kc + 1) * 128, :], w=[("win", kc)])
        identb = sb("identb", [128, 128], BF16)
        sc.dma("sp", identb[:], dr["ident_bf"], w=["identb"])
        posi = sb("posi", [128, NT], I32)
        sc.dma("sp", posi[:], dr["pos"].rearrange("(i p) o -> p (i o)", p=128), w=["posi"],
               allow_slow_non_contiguous=True)
        invf = sb("invf", [128, 8], F32)
        sc.dma("sp", invf[:], dr["invf"], w=["invf"])
        posf = sb("posf", [128, NT], F32)
        sc.copy("dve", posf[:], posi[:], r=["posi"], w=["posf"])
        ang = sb("ang", [128, NT, 8], F32)
        sc.tt("dve", ang[:], posf[:].unsqueeze(2).broadcast_to([128, NT, 8]),
              invf[:].unsqueeze(1).broadcast_to([128, NT, 8]), ALU.mult, r=["posf", "invf"], w=["ang"])
        sint = sb("sint", [128, NT, 8], F32)
        cost = sb("cost", [128, NT, 8], F32)
        tq = sb("tq", [128, NT, 8], F32)
        ki = sb("ki", [128, NT, 8], I32)
        kf = sb("kf", [128, NT, 8], F32)
        rr_ = sb("rr_", [128, NT, 8], F32)
        mm_ = sb("mm_", [128, NT, 8], F32)
        for nm, dst, off in (("s", sint, 0.0), ("c", cost, 0.5 * PI)):
            src = ang
            if off != 0.0:
                sc.ts("dve", tq[:], ang[:], off, None, ALU.add, r=["ang"], w=["tq"])
                src = tq
            sc.ts("dve", kf[:], src[:], 1.0 / (2 * PI), None, ALU.mult, r=["ang", "tq"], w=["kf"])
            sc.copy("dve", ki[:], kf[:], r=["kf"], w=["ki"])
            sc.copy("dve", kf[:], ki[:], r=["ki"], w=["kf"])
            sc.stt("dve", rr_[:], kf[:], -2 * PI, src[:], ALU.mult, ALU.add, r=["kf", "ang", "tq"], w=["rr_"])
            sc.ts("dve", mm_[:], rr_[:], PI, -2 * PI, ALU.is_gt, ALU.mult, r=["rr_"], w=["mm_"])
            sc.tt("dve", rr_[:], rr_[:], mm_[:], ALU.add, r=["rr_", "mm_"], w=["rr_"])
            sc.ts("dve", mm_[:], rr_[:], -PI, 2 * PI, ALU.is_lt, ALU.mult, r=["rr_"], w=["mm_"])
            sc.tt("dve", rr_[:], rr_[:], mm_[:], ALU.add, r=["rr_", "mm_"], w=["rr_"])
            sc.act(dst[:], rr_[:], AF.Sin, r=["rr_"], w=[nm + "int" if nm == "s" else "cost"])
        xb = [sb(f"xb{i}", [128, 4, 1024], BF16) for i in range(2)]
        xT = [sb(f"xT{i}", [128, 8, 512], BF16) for i in range(2)]
        qk = [sb(f"qk{i}", [128, 4, 2, 8, 64], BF16) for i in range(2)]
        vsb = [sb(f"vsb{i}", [128, 8, 65], BF16) for i in range(2)]
        for i in range(2):
            sc.memset("pool", vsb[i][:, :, 64:65], 1.0, w=[("vsb", i)])
        rt = sb("rt", [128, 4, 8, 8], F32)
        qst = [sb(f"qst{i}", [64, 2, 512], BF16) for i in range(3)]
        fst = [sb(f"fst{i}", [128, 512], F32) for i in range(3)]
        pT = [pm(f"pT{i}", [128, 1024], BF16) for i in range(2)]
        acc = [pm(f"acc{i}", [128, 512], F32) for i in range(3)]
        pq = [pm(f"pq{i}", [64, 1024], BF16) for i in range(2)]
        ev = ["act", "dve"]

        for J in range(NST):
            par = J % 2
            for t in range(4):
                i = 4 * J + t
                sc.dma("pool", xb[par][:, t, :], xsrc[i * 128:(i + 1) * 128, :], w=[("xb", par, t)])
            for kp in range(4):
                bi, bank = rot(pT, st, "pT")
                for kc in (2 * kp, 2 * kp + 1):
                    for t in range(4):
                        sc.tr(bank[:, (kc % 2) * 512 + t * 128:(kc % 2) * 512 + (t + 1) * 128],
                              xb[par][:, t, kc * 128:(kc + 1) * 128], identb[:],
                              r=[("xb", par, t), "identb"], w=[("pT", bi)], sig=(kc % 2 == 1 and t == 3))
                sc.copy(ev[kp % 2], xT[par][:, 2 * kp:2 * kp + 2, :].rearrange("p a b -> p (a b)"), bank[:],
                        r=[("pT", bi)], w=[("xT", par, kp)])
            for t in range(4):
                i = 4 * J + t
                for ci in range(3):
                    ai, a = rot(acc, st, "acc")
                    for kc in range(8):
                        sc.mm(a[:], xT[par][:, kc, t * 128:(t + 1) * 128], win[:, kc, ci * 512:(ci + 1) * 512],
                              start=(kc == 0), stop=(kc == 7), r=[("xT", par, kc // 2), ("win", kc)],
                              w=[("acc", ai)], sig=(kc == 7))
                    av = a[:].rearrange("p (h d) -> p h d", d=64)
                    if ci < 2:
                        dst = qk[par][:, t, ci, :, :]
                        kq = ("qk", par, t, ci)
                        cb = cost[:, i, :].unsqueeze(1).broadcast_to([128, 8, 8])
                        sn = sint[:, i, :].unsqueeze(1).broadcast_to([128, 8, 8])
                        x1 = av[:, :, 0:8]
                        x2 = av[:, :, 8:16]
                        A = ("acc", ai)
                        sc.tt("dve", rt[:, 0], x1, cb, ALU.mult, r=[A, "cost"], w=[("rt", 0)])
                        sc.tt("dve", rt[:, 1], x2, sn, ALU.mult, r=[A, "sint"], w=[("rt", 1)])
                        sc.tt("dve", rt[:, 2], x2, cb, ALU.mult, r=[A, "cost"], w=[("rt", 2)])
                        sc.tt("dve", rt[:, 3], x1, sn, ALU.mult, r=[A, "sint"], w=[("rt", 3)])
                        sc.tt("dve", dst[:, :, 0:8], rt[:, 0], rt[:, 1], ALU.subtract,
                              r=[("rt", 0), ("rt", 1)], w=[kq])
                        sc.tt("dve", dst[:, :, 8:16], rt[:, 2], rt[:, 3], ALU.add,
                              r=[("rt", 2), ("rt", 3)], w=[kq])
                        sc.copy("act", dst[:, :, 16:64], av[:, :, 16:64], r=[A], w=[kq])
                    else:
                        vi, vt = rot(vsb, st, "vsb")
                        sc.copy("act", vt[:, :, 0:64], av, r=[("acc", ai)], w=[("vsb", vi)])
                        sc.dma("sp", dr["V_d"][i * 128:(i + 1) * 128, :], vt[:].rearrange("p h d -> p (h d)"),
                               r=[("vsb", vi)], w=[("V_d", i)])
            for ci in range(2):
                for hp in range(4):
                    bi, bank = rot(pq, st, "pq")
                    for hh in range(2):
                        for t in range(4):
                            sc.tr(bank[:, hh * 512 + t * 128:hh * 512 + (t + 1) * 128],
                                  qk[par][:, t, ci, 2 * hp + hh, :], identb[:],
                                  r=[("qk", par, t, ci), "identb"], w=[("pq", bi)], sig=(hh == 1 and t == 3))
                    qi, qs = rot(qst, st, "qst")
                    sc.copy(ev[hp % 2], qs[:].rearrange("p a b -> p (a b)"), bank[:], r=[("pq", bi)],
                            w=[("qst", qi)])
                    sc.dma("sp", dr["QK_d"][ci, 2 * hp:2 * hp + 2, :, J * 512:(J + 1) * 512].rearrange(
                        "h p n -> p h n"), qs[:], r=[("qst", qi)], w=[("QK_d", ci, 2 * hp, J), ("QK_d", ci, 2 * hp + 1, J)])
            for c in range(12):
                ai, a = rot(acc, st, "acc")
                for kc in range(8):
                    sc.mm(a[:], win[:, kc, 1536 + c * 128:1536 + (c + 1) * 128], xT[par][:, kc, :],
                          start=(kc == 0), stop=(kc == 7), r=[("xT", par, kc // 2), ("win", kc)],
                          w=[("acc", ai)], sig=(kc == 7))
                fi, fs = rot(fst, st, "fst")
                sc.copy(ev[c % 2], fs[:], a[:], r=[("acc", ai)], w=[("fst", fi)])
                sc.dma("sp", dr["FM_d"][c, :, J * 512:(J + 1) * 512], fs[:], r=[("fst", fi)], w=[("FM_d", c, J)])
    sc.barrier()


def phase_attn(sc, nc, dr, heads=range(8), co=None):
    _PH[0] += 1
    with ExitStack() as ps:
        def sb(name, shape, dt):
            return ps.enter_context(nc.sbuf_tensor(f"u{_PH[0]}_p2_" + name, shape, dt))

        def pm(name, shape, dt):
            return ps.enter_context(nc.psum_tensor(f"u{_PH[0]}_p2_" + name, shape, dt))

        st = {}
        identb = sb("identb", [128, 128], BF16)
        sc.dma("sp", identb[:], dr["ident_bf"], w=["identb"])
        causal = sb("causal", [128, 4, 512], BF16)
        sc.dma("sp", causal[:], dr["causal"], w=["causal"])
        gm1 = sb("gm1", [128, 512], F32)
        gm2 = sb("gm2", [128, 512], F32)
        sc.dma("sp", gm1[:], dr["gmask1"], w=["gm1"])
        sc.dma("sp", gm2[:], dr["gmask2"], w=["gm2"])
        sel65 = sb("sel65", [65, 64], F32)
        sc.dma("sp", sel65[:], dr["sel65"], w=["sel65"])
        vall = sb("vall", [128, NT, 520], BF16)
        vsrc = dr["V_d"].rearrange("(i p) c -> p i c", p=128)
        for g in range(4):
            sc.dma("sp", vall[:, 8 * g:8 * g + 8, :], vsrc[:, 8 * g:8 * g + 8, :],
                   r=[("V_d", i) for i in range(8 * g, 8 * g + 8)], w=[("vall", g)])
        kaug = [sb(f"kaug{i}", [80, S_], BF16) for i in range(2)]
        qaug = [sb(f"qaug{i}", [80, S_], BF16) for i in range(2)]
        for b in range(2):
            sc.dma("sp", kaug[b][64:80, :], dr["blkind"], w=[("kaug", b, "c")])
        biasT = sb("biasT", [128, NT, 80], BF16)
        sc.memset("pool", biasT[:], 0.0, w=["biasT"])
        gs1 = sb("gs1", [128, 512], F32)
        gs2 = sb("gs2", [128, 512], F32)
        m8 = sb("m8", [128, NT, 8], F32)
        km = sb("km", [64, 16], F32)
        kmb = sb("kmb", [64, 16], BF16)
        gp = pm("gp", [128, 512], F32)
        bp = bc = gp
        sT = [pm(f"sT{i}", [128, 512], F32) for i in range(3)]
        oT = [pm(f"oT{i}", [128, 512], F32) for i in range(2)]
        cogen = None
        if co is not None:
            cogen = co(lambda n, sh, dt: sb("co_" + n, sh, dt), lambda n, sh, dt: pm("co_" + n, sh, dt))
        pts = [sb(f"pts{i}", [128, 512], BF16) for i in range(3)]
        osb = [sb(f"osb{i}", [65, 512], F32) for i in range(2)]
        rec = [sb(f"rec{i}", [64, 512], F32) for i in range(2)]
        ast = [sb(f"ast{i}", [64, 512], BF16) for i in range(2)]

        def head_prep(h):
            b = h % 2
            sc.dma("sp", kaug[b][0:64, :], dr["QK_d"][1, h], r=[("QK_d", 1, h, J) for J in range(NST)],
                   w=[("kaug", b)])
            sc.dma("sp", qaug[b][0:64, :], dr["QK_d"][0, h], r=[("QK_d", 0, h, J) for J in range(NST)],
                   w=[("qaug", b)])
            sc.op("dve", lambda e: e.tensor_reduce(out=km[:], in_=kaug[b][0:64, :].rearrange("p (n l) -> p n l", l=256),
                                                   axis=AX.X, op=ALU.add), r=[("kaug", b)], w=["km"])
            sc.act(kmb[:], km[:], AF.Copy, r=["km"], w=["kmb"], scale=1.0 / 256.0)
            yield
            for i in range(NT):
                sc.mm(gp[:, i * 16:(i + 1) * 16], qaug[b][0:64, i * 128:(i + 1) * 128], kmb[:],
                      r=[("qaug", b), "kmb"], w=["gbank"], sig=(i == NT - 1))
            sc.tt("dve", gs1[:], gp[:], gm1[:], ALU.add, r=["gbank", "gm1"], w=["gs1"])
            sc.tt("dve", gs2[:], gp[:], gm2[:], ALU.add, r=["gbank", "gm2"], w=["gs2"])
            yield
            for i in range(NT):
                sc.op("dve", lambda e: e.max(out=m8[:, i, :], in_=gs1[:, i * 16:(i + 1) * 16]), r=["gs1"],
                      w=[("m8", i)])
                sc.ts("dve", biasT[:, i, 64:80], gs2[:, i * 16:(i + 1) * 16], m8[:, i, 2:3], NEG, ALU.is_lt,
                      ALU.mult, r=["gs2", ("m8", i)], w=["biasT"])
                if i % 4 == 3:
                    yield
            for J in range(NST):
                for t in range(4):
                    sc.mm(bp[0:80, t * 128:(t + 1) * 128], biasT[:, 4 * J + t, :], identb[:],
                          r=["biasT", "identb"], w=["gbank"], sig=(t == 3))
                sc.copy("dve", qaug[b][64:80, J * 512:(J + 1) * 512], bp[64:80, :], r=["gbank"], w=[("qaug", b)])
                yield

        def pull(gen, n):
            if gen is None:
                return
            for _ in range(n):
                try:
                    next(gen)
                except StopIteration:
                    return

        heads = list(heads)
        pend_epi = []
        pull(head_prep(heads[0]), 10 ** 6)
        for hidx, h in enumerate(heads):
            b = h % 2
            gen = head_prep(heads[hidx + 1]) if hidx + 1 < len(heads) else None
            for J in range(NST):
                oi, o = rot(oT, st, "oT")
                nk = 4 * J + 4
                qs = qaug[b][:, J * 512:(J + 1) * 512]
                sbank = {}

                def issue_s(kt):
                    si, s = rot(sT, st, "sT")
                    sbank[kt] = (si, s)
                    diag = kt >= 4 * J
                    sc.mm(s[:], kaug[b][:, kt * 128:(kt + 1) * 128], qs, start=True, stop=not diag,
                          r=[("kaug", b), ("kaug", b, "c"), ("qaug", b)], w=[("sT", si)], sig=not diag)
                    if diag:
                        sc.mm(s[:], identb[:], causal[:, kt - 4 * J, :], start=False, stop=True,
                              r=["identb", "causal"], w=[("sT", si)])

                issue_s(0)
                if nk > 1:
                    issue_s(1)
                for kt in range(nk):
                    if kt + 2 < nk:
                        issue_s(kt + 2)
                    si, s = sbank[kt]
                    pi, p = rot(pts, st, "pts")
                    sc.act(p[:], s[:], AF.Exp, r=[("sT", si)], w=[("pts", pi)], scale=0.125)
                    sc.mm(o[0:65, :], vall[:, kt, h * 65:(h + 1) * 65], p[:], start=(kt == 0), stop=(kt == nk - 1),
                          r=[("vall", kt // 8), ("pts", pi)], w=[("oT", oi)])
                    if kt == min(2, nk - 1) and pend_epi:
                        pend_epi.pop(0)()
                    if kt % 8 == 7:
                        pull(gen, 1)
                ob, osb_ = rot(osb, st, "osb")
                sc.copy("dve", osb_[:], o[0:65, :], r=[("oT", oi)], w=[("osb", ob)])

                def epi(osb_=osb_, ob=ob, h=h, J=J):
                    sc.mm(bc[0:64, :], sel65[:], osb_[:], r=["sel65", ("osb", ob)], w=["gbank"])
                    ri, rc = rot(rec, st, "rec")
                    sc.op("dve", lambda e: e.reciprocal(out=rc[:], in_=bc[0:64, :]), r=["gbank"], w=[("rec", ri)])
                    ai, at_ = rot(ast, st, "ast")
                    sc.tt("dve", at_[:], osb_[0:64, :], rc[:], ALU.mult, r=[("osb", ob), ("rec", ri)],
                          w=[("ast", ai)])
                    sc.dma("sp", dr["ATT_d"][h, :, J * 512:(J + 1) * 512], at_[:], r=[("ast", ai)],
                           w=[("ATT_d", h, J)])

                pend_epi.append(epi)
                pull(cogen, 1)
            pull(gen, 10 ** 6)
        while pend_epi:
            pend_epi.pop(0)()
        pull(cogen, 10 ** 6)
    sc.barrier()


def colvec(sc, dst, src128, wkey):
    sc.dma("sp", dst, src128.rearrange("(p o) -> p o", o=1), w=[wkey])


def lru_co(sc, nc, dr, l, sb, pm, npsum=2):
    if True:
        st = {}
        PL = 1024
        cw = sb("cw", [128, 2, 4], F32)
        par = sb("par", [128, 2, 4], F32)
        for cc in range(2):
            for j in range(4):
                colvec(sc, cw[:, cc, j:j + 1], dr["lru_conv_w"][l, j, cc * 128:(cc + 1) * 128], "cw")
            for j, nm in enumerate(["lru_conv_b", "lru_ga_b", "lru_gx_b", "lru_lambda"]):
                colvec(sc, par[:, cc, j:j + 1], dr[nm][l, cc * 128:(cc + 1) * 128], "par")
        wa = sb("wa", [128, 2, 128], F32)
        wx = sb("wx", [128, 2, 128], F32)
        sc.memset("pool", wa[:], 0.0, w=["wa"])
        sc.memset("pool", wx[:], 0.0, w=["wx"])
        for cc in range(2):
            for hh in range(2):
                sc.dma("sp", wa[hh * 64:(hh + 1) * 64, cc, hh * 64:(hh + 1) * 64], dr["lru_ga_w"][l, 2 * cc + hh],
                       w=["wa"])
                sc.dma("sp", wx[hh * 64:(hh + 1) * 64, cc, hh * 64:(hh + 1) * 64], dr["lru_gx_w"][l, 2 * cc + hh],
                       w=["wx"])
        cvt = sb("cvt", [128, 2], F32)
        cvec = sb("cvec", [128, 2], F32)
        sc.act(cvt[:], par[:, :, 3], AF.Exp, r=["par"], w=["cvt"], scale=-1.0)
        sc.act(cvec[:], cvt[:], AF.Ln, r=["cvt"], w=["cvec0"], bias=1.0)
        sc.ts("dve", cvec[:], cvec[:], -8.0, None, ALU.mult, r=["cvec0"], w=["cvec"])
        hc = sb("hc", [128, 2], F32)
        sc.memset("dve", hc[:], 0.0, w=["hc"])
        xh = [sb(f"xh{i}", [128, 3 + PL], F32) for i in range(2)]
        gl = [sb(f"gl{i}", [128, PL], F32) for i in range(2)]
        xc = sb("xc", [128, PL], F32)
        rr = sb("rr", [128, PL], F32)
        ii = sb("ii", [128, PL], F32)
        aa = sb("aa", [128, PL], F32)
        t1 = sb("t1", [128, PL], F32)
        uu = sb("uu", [128, PL], F32)
        hh_ = sb("hh", [128, PL], F32)
        ge = sb("ge", [128, PL], F32)
        yo = [sb(f"yo{i}", [128, PL], BF16) for i in range(2)]
        pa = [pm(f"pa{i}", [128, 512], F32) for i in range(npsum)]
        px = [pm(f"px{i}", [128, 512], F32) for i in range(npsum)]
        n = 0
        for cc in range(2):
            for pc in range(S_ // PL):
                xi, xh_ = rot(xh, st, "xh")
                gi, gl_ = rot(gl, st, "gl")
                JJ = [2 * pc, 2 * pc + 1]
                if pc == 0:
                    sc.memset("pool", xh_[:, 0:3], 0.0, w=[("xh", xi)])
                    sc.dma("act", xh_[:, 3:3 + PL], dr["FM_d"][cc, :, 0:PL], r=[("FM_d", cc, J) for J in JJ],
                           w=[("xh", xi)])
                else:
                    sc.dma("act", xh_[:, :], dr["FM_d"][cc, :, pc * PL - 3:(pc + 1) * PL],
                           r=[("FM_d", cc, J) for J in JJ + [2 * pc - 1]], w=[("xh", xi)])
                sc.dma("act", gl_[:], dr["FM_d"][2 + cc, :, pc * PL:(pc + 1) * PL], r=[("FM_d", 2 + cc, J) for J in JJ],
                       w=[("gl", gi)])
                X = ("xh", xi)
                sc.ts("dve", xc[:], xh_[:, 0:PL], cw[:, cc, 0:1], par[:, cc, 0:1], ALU.mult, ALU.add,
                      r=[X, "cw", "par"], w=["xc"])
                for j in range(1, 4):
                    sc.stt("dve", xc[:], xh_[:, j:j + PL], cw[:, cc, j:j + 1], xc[:], ALU.mult, ALU.add,
                           r=[X, "cw", "xc"], w=["xc"])
                yield
                for hf in range(PL // 512):
                    sl = slice(hf * 512, (hf + 1) * 512)
                    ai, a = rot(pa, st, "pa")
                    bi, bx = rot(px, st, "px")
                    sc.mm(a[:], wa[:, cc, :], xc[:, sl], r=["wa", "xc"], w=[("pa", ai)])
                    sc.mm(bx[:], wx[:, cc, :], xc[:, sl], r=["wx", "xc"], w=[("px", bi)])
                    sc.act(rr[:, sl], a[:], AF.Sigmoid, r=[("pa", ai), "par"], w=["rr"], bias=par[:, cc, 1:2])
                    sc.act(ii[:, sl], bx[:], AF.Sigmoid, r=[("px", bi), "par"], w=["ii"], bias=par[:, cc, 2:3])
                yield
                sc.act(aa[:], rr[:], AF.Exp, r=["rr", "cvec"], w=["aa"], scale=cvec[:, cc:cc + 1])
                sc.tt("dve", t1[:], aa[:], aa[:], ALU.mult, r=["aa"], w=["t1"])
                sc.act(t1[:], t1[:], AF.Sqrt, r=["t1"], w=["t1"], scale=-1.0, bias=1.0)
                sc.tt("dve", uu[:], ii[:], xc[:], ALU.mult, r=["ii", "xc"], w=["uu"])
                sc.tt("dve", uu[:], uu[:], t1[:], ALU.mult, r=["uu", "t1"], w=["uu"])
                yield
                init = 0.0 if pc == 0 else hc[:, cc:cc + 1]
                sc.op("dve", lambda e: e.tensor_tensor_scan(out=hh_[:], data0=aa[:], data1=uu[:], initial=init,
                                                            op0=ALU.mult, op1=ALU.add),
                      r=["aa", "uu", "hc"], w=["hh"])
                sc.copy("dve", hc[:, cc:cc + 1], hh_[:, PL - 1:PL], r=["hh"], w=["hc"])
                yield
                G = ("gl", gi)
                sc.tt("pool", ge[:], gl_[:], gl_[:], ALU.mult, r=[G], w=["ge"])
                sc.ts("pool", ge[:], ge[:], 0.044715, 1.0, ALU.mult, ALU.add, r=["ge"], w=["ge"])
                sc.tt("pool", ge[:], ge[:], gl_[:], ALU.mult, r=["ge", G], w=["ge"])
                sc.act(ge[:], ge[:], AF.Sigmoid, r=["ge"], w=["ge"], scale=1.5957691216)
                sc.tt("pool", ge[:], ge[:], gl_[:], ALU.mult, r=["ge", G], w=["ge"])
                yield
                yi, y = rot(yo, st, "yo")
                sc.tt("dve", y[:], hh_[:], ge[:], ALU.mult, r=["hh", "ge"], w=[("yo", yi)])
                for J in JJ:
                    o0 = (J - JJ[0]) * 512
                    sc.dma("sp", dr["LR_d"][cc, :, J * 512:(J + 1) * 512], y[:, o0:o0 + 512], r=[("yo", yi)],
                           w=[("LR_d", cc, J)])
            yield


def phase_lru(sc, nc, dr, l):
    _PH[0] += 1
    with ExitStack() as ps:
        def sb(name, shape, dt):
            return ps.enter_context(nc.sbuf_tensor(f"u{_PH[0]}_p3_" + name, shape, dt))

        def pm(name, shape, dt):
            return ps.enter_context(nc.psum_tensor(f"u{_PH[0]}_p3_" + name, shape, dt))

        for _ in lru_co(sc, nc, dr, l, sb, pm):
            pass
    sc.barrier()


def phase_rwkv(sc, nc, dr, l):
    _PH[0] += 1
    C0 = math.exp(-0.5)
    with ExitStack() as ps:
        def sb(name, shape, dt=F32):
            return ps.enter_context(nc.sbuf_tensor(f"u{_PH[0]}_p4_" + name, shape, dt))

        st = {}
        pbank = [ps.enter_context(nc.psum_tensor(f"u{_PH[0]}_p4_pb{i}", [128, 512], F32)) for i in range(8)]

        def bank():
            i, b = rot(pbank, st, "pb")
            return ("pb", i), b

        identf = sb("identf", [128, 128])
        sc.dma("sp", identf[:], dr["ident_f"], w=["identf"])
        rmask = sb("rmask", [128, 512])
        sc.dma("sp", rmask[:], dr["rmask"], w=["rmask"])
        mtmask = sb("mtmask", [64, 256])
        sc.dma("sp", mtmask[:], dr["mtmask"], w=["mtmask"])
        slmask = sb("slmask", [64, 64])
        sc.dma("sp", slmask[:], dr["slmask"], w=["slmask"])
        bones = sb("bones", [128, 128])
        sc.dma("sp", bones[:], dr["bones"], w=["bones"])
        hsel = sb("hsel", [128, 2])
        sc.dma("sp", hsel[:], dr["hsel"], w=["hsel"])
        mu = sb("mu", [128, 8])
        omm = sb("omm", [128, 8])
        for c in range(8):
            colvec(sc, mu[:, c:c + 1], dr["rwkv_mu"][l, c * 128:(c + 1) * 128], "mu")
        sc.ts("dve", omm[:], mu[:], -1.0, 1.0, ALU.mult, ALU.add, r=["mu"], w=["omm"])
        pp = sb("pp", [128, 2, 6])
        rkf = dr["rwkv_r_k"][l].rearrange("h d -> (h d)")
        for cc in range(2):
            for j, src in enumerate([dr["rwkv_w0"][l], dr["rwkv_a0"][l], dr["rwkv_k_k"][l], dr["rwkv_k_a"][l]]):
                colvec(sc, pp[:, cc, j:j + 1], src[cc * 128:(cc + 1) * 128], "pp")
            colvec(sc, pp[:, cc, 5:6], rkf[cc * 128:(cc + 1) * 128], "pp")
        sc.ts("dve", pp[:, :, 4], pp[:, :, 3], -1.0, 1.0, ALU.mult, ALU.add, r=["pp"], w=["pp"])
        wup = sb("wup", [128, 256])
        sc.dma("sp", wup[0:64, :], dr["rwkv_w_up"][l], w=["wup"])
        sc.dma("sp", wup[64:128, :], dr["rwkv_a_up"][l], w=["wup"])
        gup = sb("gup", [128, 256])
        sc.dma("sp", gup[:], dr["rwkv_g_up"][l], w=["gup"])
        lng = sb("lng", [64, 256])
        lnb = sb("lnb", [64, 256])
        sc.dma("sp", lng[:], dr["rwkv_lnx_g"][l].partition_broadcast(64), w=["lng"])
        sc.dma("sp", lnb[:], dr["rwkv_lnx_b"][l].partition_broadcast(64), w=["lnb"])
        ST = sb("ST", [128, 2, 64])
        sc.memset("dve", ST[:], 0.0, w=[("ST", 0), ("ST", 1)])
        STb = sb("STb", [128, 2, 64], BF16)
        sc.memset("dve", STb[:], 0.0, w=[("STb", 0), ("STb", 1)])
        ptc = [sb(f"ptc{i}", [128, 513]) for i in range(2)]
        pf = sb("pf", [128, 8, 512])
        txw = sb("txw", [64, 512])
        sxg = sb("sxg", [128, 512])
        names = ["lw", "cum", "g_", "ginv", "gprev", "ginvC", "a_", "kk", "tA", "kp", "b_", "rk"]
        T = {n: sb(n, [128, 512]) for n in names}
        T["bh"] = T["g_"]
        T["kh"] = T["ginv"]
        AR = sb("AR", [128, 8, 128], BF16)
        BK = sb("BK", [128, 8, 128], BF16)
        BKz = [sb(f"BKz{i}", [128, 8, 128], BF16) for i in range(2)]
        PQ = [sb(f"PQ{i}", [64, 16, 64], BF16) for i in range(4)]
        TT = [sb(f"TT{i}", [64, 16, 64], BF16) for i in range(2)]
        UB = [(jb, cc) for jb in range(2) for cc in range(2)]
        ARz = {u: [sb(f"ARz{u[0]}{u[1]}_{i}", [128, 8, 128], BF16) for i in range(2)] for u in UB}
        MTu = {u: sb(f"MT{u[0]}{u[1]}", [64, 16, 256], BF16) for u in UB}
        TTf = {u: sb(f"TTf{u[0]}{u[1]}", [64, 16, 64], BF16) for u in UB}
        TOKu = {u: {n: sb(f"tok{u[0]}{u[1]}_" + n, [64, 8, 128], BF16) for n in ("bh", "kh", "v")} for u in UB}
        GTu = {u: sb(f"GT{u[0]}{u[1]}", [64, 8, 128]) for u in UB}
        RKu = {u: sb(f"RK{u[0]}{u[1]}", [64, 8, 2]) for u in UB}
        gCu = {u: sb(f"gC{u[0]}{u[1]}", [128, 8]) for u in UB}
        for u in UB:
            for i in range(2):
                sc.memset("pool", ARz[u][i][:], 0.0, w=[("ARz", u)])
        for i in range(2):
            sc.memset("pool", BKz[i][:], 0.0, w=["BKz"])
        Zs = sb("Zs", [64, 256], BF16)
        Us = sb("Us", [64, 256], BF16)
        Ysb = sb("Ysb", [64, 8, 256])
        ysq = sb("ysq", [64, 8, 256])
        yo = sb("yo", [64, 8, 256])
        sstat = sb("sstat", [64, 6, 32])
        ost = [sb(f"ost{i}", [128, 512], BF16) for i in range(2)]
        v3 = lambda ap: ap.rearrange("p (c t) -> p c t", t=64)

        def unit_A(J, cc, ub):
            MT, TOK, GT, RK, gC = MTu[ub], TOKu[ub], GTu[ub], RKu[ub], gCu[ub]
            if cc == 0:
                for c in range(8):
                    pi, p_ = rot(ptc, st, "ptc")
                    if J == 0:
                        sc.memset("pool", p_[:, 0:1], 0.0, w=[("ptc", pi)])
                        sc.dma("sp", p_[:, 1:513], dr["FM_d"][4 + c, :, 0:512], r=[("FM_d", 4 + c, 0)], w=[("ptc", pi)])
                    else:
                        sc.dma("sp", p_[:, :], dr["FM_d"][4 + c, :, J * 512 - 1:(J + 1) * 512],
                               r=[("FM_d", 4 + c, J), ("FM_d", 4 + c, J - 1)], w=[("ptc", pi)])
                    tsh = T["tA"] if c % 2 == 0 else T["rk"]
                    tkey = "tA" if c % 2 == 0 else "rk"
                    sc.act(tsh[:], p_[:, 1:513], AF.Copy, r=[("ptc", pi), "omm"], w=[tkey], scale=omm[:, c:c + 1])
                    sc.stt("dve", pf[:, c, :], p_[:, 0:512], mu[:, c:c + 1], tsh[:], ALU.mult, ALU.add,
                           r=[("ptc", pi), "mu", tkey], w=[("pf", c)])
                    yield
                sc.act(txw[:], pf[0:64, 6, :], AF.Tanh, r=[("pf", 6)], w=["txw"])
                sc.act(sxg[:], pf[:, 7, :], AF.Sigmoid, r=[("pf", 7)], w=["sxg"])
                yield
            r_ = pf[:, cc, :]
            k_ = pf[:, 2 + cc, :]
            v_ = pf[:, 4 + cc, :]
            RKEY, KKEY, VKEY = ("pf", cc), ("pf", 2 + cc), ("pf", 4 + cc)
            bk, b = bank()
            sc.mm(b[:], wup[0:64, cc * 128:(cc + 1) * 128], txw[:], r=["wup", "txw"], w=[bk])
            sc.act(T["lw"][:], b[:], AF.Sigmoid, r=[bk, "pp"], w=["lw"], bias=pp[:, cc, 0:1])
            bk, b = bank()
            sc.mm(b[:], wup[64:128, cc * 128:(cc + 1) * 128], pf[64:128, 6, :], r=["wup", ("pf", 6)], w=[bk])
            sc.act(T["a_"][:], b[:], AF.Sigmoid, r=[bk, "pp"], w=["a_"], bias=pp[:, cc, 1:2])
            yield
            sc.op("dve", lambda e: e.tensor_tensor_scan(out=T["cum"][:], data0=rmask[:], data1=T["lw"][:],
                                                        initial=0.0, op0=ALU.mult, op1=ALU.add),
                  r=["rmask", "lw"], w=["cum"])
            sc.act(T["g_"][:], T["cum"][:], AF.Exp, r=["cum"], w=["g_"], scale=-C0)
            sc.act(T["ginv"][:], T["cum"][:], AF.Exp, r=["cum"], w=["ginv"], scale=C0)
            sc.tt("dve", T["tA"][:], T["cum"][:], T["lw"][:], ALU.subtract, r=["cum", "lw"], w=["tA"])
            sc.act(T["gprev"][:], T["tA"][:], AF.Exp, r=["tA"], w=["gprev"], scale=-C0)
            yield
            cumC = T["cum"][:, 63::64]
            sc.tt("dve", v3(T["tA"][:]), cumC.unsqueeze(2).broadcast_to([128, 8, 64]), v3(T["cum"][:]),
                  ALU.subtract, r=["cum", "gprev"], w=["tA"])
            sc.act(T["ginvC"][:], T["tA"][:], AF.Exp, r=["tA"], w=["ginvC"], scale=-C0)
            sc.act(gC[:], cumC, AF.Exp, r=["cum"], w=[("gC", ub)], scale=-C0)
            yield
            sc.ts("dve", T["kk"][:], k_, pp[:, cc, 2:3], None, ALU.mult, r=[KKEY, "pp"], w=["kk"])
            sc.tt("dve", T["tA"][:], T["kk"][:], T["kk"][:], ALU.mult, r=["kk", "ginvC"], w=["tA"])
            bk, b = bank()
            sc.mm(b[:], bones[:], T["tA"][:], r=["bones", "tA"], w=[bk])
            sc.ts("dve", T["tA"][:], b[:], 1e-24, None, ALU.max, r=[bk], w=["tA"])
            sc.act(T["tA"][:], T["tA"][:], AF.Ln, r=["tA"], w=["tA"])
            sc.act(T["tA"][:], T["tA"][:], AF.Exp, r=["tA"], w=["tA"], scale=-0.5)
            sc.tt("dve", T["kk"][:], T["kk"][:], T["tA"][:], ALU.mult, r=["kk", "tA"], w=["kk"])
            yield
            sc.ts("dve", T["tA"][:], T["a_"][:], pp[:, cc, 3:4], pp[:, cc, 4:5], ALU.mult, ALU.add,
                  r=["a_", "pp", "kk"], w=["tA"])
            sc.tt("dve", T["kp"][:], T["tA"][:], k_, ALU.mult, r=["tA", KKEY], w=["kp"])
            sc.tt("dve", T["b_"][:], T["kk"][:], T["a_"][:], ALU.mult, r=["kk", "a_"], w=["b_"])
            sc.stt("dve", T["rk"][:], r_, pp[:, cc, 5:6], T["kp"][:], ALU.mult, ALU.mult, r=[RKEY, "pp", "kp"],
                   w=["rk"])
            yield
            sc.stt("dve", AR[:, :, 0:64], v3(T["kk"][:]), -1.0, v3(T["gprev"][:]), ALU.mult, ALU.mult,
                   r=["kk", "gprev"], w=["AR"])
            sc.tt("dve", AR[:, :, 64:128], v3(r_), v3(T["g_"][:]), ALU.mult, r=[RKEY, "g_"], w=["AR"])
            sc.tt("dve", BK[:, :, 0:64], v3(T["b_"][:]), v3(T["ginv"][:]), ALU.mult, r=["b_", "ginv"], w=["BK"])
            sc.tt("dve", BK[:, :, 64:128], v3(T["kp"][:]), v3(T["ginv"][:]), ALU.mult, r=["kp", "ginv"], w=["BK"])
            yield
            for i in range(2):
                hs_ = slice(i * 64, (i + 1) * 64)
                sc.copy("act", ARz[ub][i][hs_, :, :], AR[hs_, :, :], r=["AR"], w=[("ARz", ub)])
                sc.copy("dve", BKz[i][hs_, :, :], BK[hs_, :, :], r=["BK"], w=["BKz"])
            sc.tt("pool", T["bh"][:], T["b_"][:], T["ginvC"][:], ALU.mult, r=["b_", "ginvC", "g_", "AR"], w=["g_"])
            sc.tt("pool", T["kh"][:], T["kp"][:], T["ginvC"][:], ALU.mult, r=["kp", "ginvC", "ginv", "BK"],
                  w=["ginv"])
            yield
            for nm, src, skey in (("bh", T["bh"][:], "g_"), ("kh", T["kh"][:], "ginv"), ("v", v_, VKEY)):
                for q4 in range(2):
                    bk, b = bank()
                    for c4 in range(4):
                        c = q4 * 4 + c4
                        sc.tr(b[0:64, c4 * 128:(c4 + 1) * 128], src[:, c * 64:(c + 1) * 64], identf[:],
                              r=[skey, "identf"], w=[bk], sig=(c4 == 3))
                    sc.copy("act", TOK[nm][:, q4 * 4:q4 * 4 + 4, :], b[0:64, :].rearrange("p (c n) -> p c n", n=128),
                            r=[bk], w=[("tok", ub, nm)])
                    yield
            for q4 in range(2):
                bk, b = bank()
                for c4 in range(4):
                    c = q4 * 4 + c4
                    sc.mm(b[0:64, c4 * 128:(c4 + 1) * 128], sxg[:, c * 64:(c + 1) * 64],
                          gup[:, cc * 128:(cc + 1) * 128], r=["sxg", "gup"], w=[bk], sig=(c4 == 3))
                sc.copy("act", GT[:, q4 * 4:q4 * 4 + 4, :], b[0:64, :].rearrange("p (c n) -> p c n", n=128), r=[bk],
                        w=[("GT", ub)])
                yield
            bk, b = bank()
            for c in range(8):
                sc.mm(b[0:64, c * 2:c * 2 + 2], T["rk"][:, c * 64:(c + 1) * 64], hsel[:], r=["rk", "hsel"], w=[bk],
                      sig=(c == 7))
            sc.copy("dve", RK[:].rearrange("p c h -> p (c h)"), b[0:64, 0:16], r=[bk], w=[("RK", ub)])
            yield
            for jp in range(8):
                bk, b = bank()
                for jj in range(2):
                    j = jp * 2 + jj
                    c, hh = j // 2, j % 2
                    sc.mm(b[0:64, jj * 256:jj * 256 + 128], BKz[hh][:, c, 0:64], AR[:, c, :], r=["BKz", "AR"], w=[bk],
                          sig=False)
                    sc.mm(b[0:64, jj * 256 + 128:jj * 256 + 256], BKz[hh][:, c, 64:128], AR[:, c, :], r=["BKz", "AR"],
                          w=[bk], sig=(jj == 1))
                sc.tt("dve", MT[:, jp * 2:jp * 2 + 2, :], b[0:64, :].rearrange("p (a n) -> p a n", n=256),
                      mtmask[:].unsqueeze(1).broadcast_to([64, 2, 256]), ALU.mult, r=[bk, "mtmask"],
                      w=[("MT", ub, jp // 4)])
                yield
            P, Q, Pn, Qn = PQ
            for g8 in range(2):
                bk, b = bank()
                for j8 in range(8):
                    j = g8 * 8 + j8
                    c, hh = j // 2, j % 2
                    sc.mm(b[0:64, j8 * 64:(j8 + 1) * 64], ARz[ub][hh][:, c, 0:64], BK[:, c, 0:64],
                          r=[("ARz", ub), "BK"], w=[bk], sig=(j8 == 7))
                sc.tt("dve", P[:, g8 * 8:(g8 + 1) * 8, :], b[0:64, :].rearrange("p (a n) -> p a n", n=64),
                      slmask[:].unsqueeze(1).broadcast_to([64, 8, 64]), ALU.mult, r=[bk, "slmask"],
                      w=[("P", id(P), g8)])
                sc.copy("act", Q[:, g8 * 8:(g8 + 1) * 8, :], MT[:, g8 * 8:(g8 + 1) * 8, 0:64], r=[("MT", ub, g8)],
                        w=[("P", id(Q), g8)])
                sc.tt("dve", TT[0][:, g8 * 8:(g8 + 1) * 8, :], MT[:, g8 * 8:(g8 + 1) * 8, 0:64],
                      identf[0:64, 0:64].unsqueeze(1).broadcast_to([64, 8, 64]), ALU.add,
                      r=[("MT", ub, g8), "identf"], w=[("TT", 0, g8)])
                yield
            tcur = 0
            for lvl in range(1, 6):
                for g8 in range(2):
                    bk, b = bank()
                    for j8 in range(8):
                        j = g8 * 8 + j8
                        sc.mm(b[0:64, j8 * 64:(j8 + 1) * 64], Q[:, j, :], P[:, j, :],
                              r=[("P", id(P), g8), ("P", id(Q), g8)], w=[bk], sig=(j8 == 7))
                    sc.copy("act", Pn[:, g8 * 8:(g8 + 1) * 8, :], b[0:64, :].rearrange("p (a n) -> p a n", n=64),
                            r=[bk], w=[("P", id(Pn), g8)])
                    yield
                    if lvl < 5:
                        bk, b = bank()
                        for j8 in range(8):
                            j = g8 * 8 + j8
                            sc.mm(b[0:64, j8 * 64:(j8 + 1) * 64], P[:, j, :], Q[:, j, :],
                                  r=[("P", id(P), g8), ("P", id(Q), g8)], w=[bk], sig=(j8 == 7))
                        sc.copy("dve", Qn[:, g8 * 8:(g8 + 1) * 8, :],
                                b[0:64, :].rearrange("p (a n) -> p a n", n=64), r=[bk], w=[("P", id(Qn), g8)])
                        yield
                    bk, b = bank()
                    for j8 in range(8):
                        j = g8 * 8 + j8
                        sc.mm(b[0:64, j8 * 64:(j8 + 1) * 64], Pn[:, j, :], TT[tcur][:, j, :],
                              r=[("P", id(Pn), g8), ("TT", tcur, g8)], w=[bk], sig=(j8 == 7))
                    dstT = TTf[ub] if lvl == 5 else TT[1 - tcur]
                    dkey = ("TTf", ub, g8) if lvl == 5 else ("TT", 1 - tcur, g8)
                    sc.tt("dve", dstT[:, g8 * 8:(g8 + 1) * 8, :],
                          b[0:64, :].rearrange("p (a n) -> p a n", n=64), TT[tcur][:, g8 * 8:(g8 + 1) * 8, :],
                          ALU.add, r=[bk, ("TT", tcur, g8)], w=[dkey])
                    yield
                P, Q, Pn, Qn = Pn, Qn, P, Q
                tcur = 1 - tcur

        def pull(gen, n):
            if gen is None:
                return
            for _ in range(n):
                try:
                    next(gen)
                except StopIteration:
                    return

        def unit_B(J, jb, gen):
            for c in range(8):
                g8 = c // 4
                kz, bz = bank()
                for cc in range(2):
                    ub = (jb, cc)
                    for hh in range(2):
                        j = c * 2 + hh
                        hd = cc * 2 + hh
                        sc.mm(bz[0:64, hd * 64:(hd + 1) * 64], ARz[ub][hh][:, c, 0:64], STb[:, cc, :], start=True,
                              stop=False, r=[("ARz", ub), ("STb", cc)], w=[kz], sig=False)
                        sc.mm(bz[0:64, hd * 64:(hd + 1) * 64], MTu[ub][:, j, 128:192],
                              TOKu[ub]["v"][:, c, hh * 64:(hh + 1) * 64], start=False, stop=True,
                              r=[("MT", ub, g8), ("tok", ub, "v")], w=[kz], sig=(hd == 3))
                sc.copy("act", Zs[:], bz[0:64, 0:256], r=[kz], w=["Zs"])
                pull(gen, 5)
                ku, bu = bank()
                for cc in range(2):
                    ub = (jb, cc)
                    for hh in range(2):
                        j = c * 2 + hh
                        hd = cc * 2 + hh
                        sc.mm(bu[0:64, hd * 64:(hd + 1) * 64], TTf[ub][:, j, :], Zs[:, hd * 64:(hd + 1) * 64],
                              r=[("TTf", ub, g8), "Zs"], w=[ku], sig=(hd == 3))
                sc.copy("dve", Us[:], bu[0:64, 0:256], r=[ku], w=["Us"])
                pull(gen, 5)
                ky, by = bank()
                for cc in range(2):
                    ub = (jb, cc)
                    for hh in range(2):
                        j = c * 2 + hh
                        hd = cc * 2 + hh
                        o = by[0:64, hd * 64:(hd + 1) * 64]
                        sc.mm(o, ARz[ub][hh][:, c, 64:128], STb[:, cc, :], start=True, stop=False,
                              r=[("ARz", ub), ("STb", cc)], w=[ky], sig=False)
                        sc.mm(o, MTu[ub][:, j, 64:128], Us[:, hd * 64:(hd + 1) * 64], start=False, stop=False,
                              r=[("MT", ub, g8), "Us"], w=[ky], sig=False)
                        sc.mm(o, MTu[ub][:, j, 192:256], TOKu[ub]["v"][:, c, hh * 64:(hh + 1) * 64], start=False,
                              stop=True, r=[("MT", ub, g8), ("tok", ub, "v")], w=[ky], sig=(hd == 3))
                sc.copy("act", Ysb[:, c, :], by[0:64, 0:256], r=[ky], w=["Ysb"])
                kd, bd = bank()
                for cc in range(2):
                    ub = (jb, cc)
                    for hh in range(2):
                        hd = cc * 2 + hh
                        o = bd[:, hd * 64:(hd + 1) * 64]
                        sc.mm(o, TOKu[ub]["bh"][:, c, :], Us[:, hd * 64:(hd + 1) * 64], start=True, stop=False,
                              r=[("tok", ub, "bh"), "Us"], w=[kd], sig=False)
                        sc.mm(o, TOKu[ub]["kh"][:, c, :], TOKu[ub]["v"][:, c, hh * 64:(hh + 1) * 64], start=False,
                              stop=True, r=[("tok", ub, "kh"), ("tok", ub, "v")], w=[kd], sig=(hd == 3))
                for cc in range(2):
                    ub = (jb, cc)
                    for hh in range(2):
                        hd = cc * 2 + hh
                        hs = slice(hh * 64, (hh + 1) * 64)
                        sc.stt("dve", STb[hs, cc, :], ST[hs, cc, :], gCu[ub][hs, c:c + 1],
                               bd[hs, hd * 64:(hd + 1) * 64], ALU.mult, ALU.add, r=[("ST", cc), ("gC", ub), kd],
                               w=[("STb", cc)])
                        sc.stt("dve", ST[hs, cc, :], ST[hs, cc, :], gCu[ub][hs, c:c + 1],
                               bd[hs, hd * 64:(hd + 1) * 64], ALU.mult, ALU.add, r=[("ST", cc), ("gC", ub), kd],
                               w=[("ST", cc)])
                pull(gen, 6)
            Y3 = Ysb[:].rearrange("p c (h d) -> p (c h) d", d=64)
            S3 = ysq[:].rearrange("p c (h d) -> p (c h) d", d=64)
            O3 = yo[:].rearrange("p c (h d) -> p (c h) d", d=64)
            bc = lambda ap: ap.unsqueeze(2).broadcast_to([64, 32, 64])
            sc.op("dve", lambda e: e.tensor_reduce(out=sstat[:, 0, :], in_=Y3, axis=AX.X, op=ALU.add), r=["Ysb"],
                  w=["ss0"])
            sc.act(ysq[:], Ysb[:], AF.Square, r=["Ysb"], w=["ysq"])
            sc.op("dve", lambda e: e.tensor_reduce(out=sstat[:, 1, :], in_=S3, axis=AX.X, op=ALU.add), r=["ysq"],
                  w=["ss1"])
            sc.ts("dve", sstat[:, 2, :], sstat[:, 0, :], 1.0 / 64, None, ALU.mult, r=["ss0"], w=["ss2"])
            sc.tt("dve", sstat[:, 3, :], sstat[:, 2, :], sstat[:, 2, :], ALU.mult, r=["ss2"], w=["ss3"])
            sc.stt("dve", sstat[:, 4, :], sstat[:, 1, :], 1.0 / 64, sstat[:, 3, :], ALU.mult, ALU.subtract,
                   r=["ss1", "ss3"], w=["ss4"])
            sc.ts("dve", sstat[:, 4, :], sstat[:, 4, :], 64e-5, None, ALU.add, r=["ss4"], w=["ss4"])
            sc.act(sstat[:, 5, :], sstat[:, 4, :], AF.Ln, r=["ss4"], w=["ss5"])
            sc.act(sstat[:, 5, :], sstat[:, 5, :], AF.Exp, r=["ss5"], w=["ss5"], scale=-0.5)
            pull(gen, 4)
            sc.tt("dve", O3, Y3, bc(sstat[:, 2, :]), ALU.subtract, r=["Ysb", "ss2"], w=["yo"])
            sc.tt("dve", O3, O3, bc(sstat[:, 5, :]), ALU.mult, r=["yo", "ss5"], w=["yo"])
            sc.tt("dve", yo[:], yo[:], lng[:, :].unsqueeze(1).broadcast_to([64, 8, 256]), ALU.mult,
                  r=["yo", "lng"], w=["yo"])
            sc.tt("dve", yo[:], yo[:], lnb[:, :].unsqueeze(1).broadcast_to([64, 8, 256]), ALU.add,
                  r=["yo", "lnb"], w=["yo"])
            for cc in range(2):
                ub = (jb, cc)
                gsl = slice(cc * 128, (cc + 1) * 128)
                S3c = ysq[:, :, gsl].rearrange("p c (h d) -> p c h d", d=64)
                V3c = TOKu[ub]["v"][:].rearrange("p c (h d) -> p c h d", d=64)
                sc.tt("dve", S3c, V3c, RKu[ub][:].unsqueeze(3).broadcast_to([64, 8, 2, 64]), ALU.mult,
                      r=[("tok", ub, "v"), ("RK", ub), "ysq"], w=["ysq"])
            sc.tt("dve", yo[:], yo[:], ysq[:], ALU.add, r=["yo", "ysq"], w=["yo"])
            for cc in range(2):
                ub = (jb, cc)
                gsl = slice(cc * 128, (cc + 1) * 128)
                sc.tt("dve", yo[:, :, gsl], yo[:, :, gsl], GTu[ub][:], ALU.mult, r=["yo", ("GT", ub)], w=["yo"])
            pull(gen, 4)
            for cc in range(2):
                gsl = slice(cc * 128, (cc + 1) * 128)
                bk, b = bank()
                for c in range(8):
                    sc.tr(b[:, c * 64:(c + 1) * 64], yo[:, c, gsl], identf[0:64, 0:64], r=["yo", "identf"], w=[bk],
                          sig=(c == 7))
                oi, o = rot(ost, st, "ost")
                sc.copy("act", o[:], b[:], r=[bk], w=[("ost", oi)])
                sc.dma("sp", dr["LR_d"][2 + cc, :, J * 512:(J + 1) * 512], o[:], r=[("ost", oi)],
                       w=[("LR_d", 2 + cc, J)])

        def gen_A(J):
            jb = J % 2
            yield from unit_A(J, 0, (jb, 0))
            yield from unit_A(J, 1, (jb, 1))

        pull(gen_A(0), 10 ** 6)
        for J in range(NST):
            gen = gen_A(J + 1) if J + 1 < NST else None
            unit_B(J, J % 2, gen)
            pull(gen, 10 ** 6)
    sc.barrier()


def layer_norm_tile(sc, y, xo, gt, bt, sm, junk, eps, keys_r, key_w, gkeys, tag=0):
    K = lambda n: ("ln", n, tag)
    sc.memset("pool", sm[:, 0:2], 0.0, w=[K("sm"), K("s1"), K("s2")])
    sc.act(junk[:], y, AF.Copy, r=list(keys_r) + [K("sm")], w=[K("junk"), K("s1")], accum_out=sm[:, 0:1])
    sc.act(junk[:], y, AF.Square, r=list(keys_r) + [K("sm"), K("junk")], w=[K("junk"), K("s2")], accum_out=sm[:, 1:2])
    sc.ts("dve", sm[:, 2:3], sm[:, 0:1], 1.0 / 1024, None, ALU.mult, r=[K("s1")], w=[K("mean")])
    sc.tt("dve", sm[:, 3:4], sm[:, 2:3], sm[:, 2:3], ALU.mult, r=[K("mean")], w=[K("msq")])
    sc.stt("dve", sm[:, 4:5], sm[:, 1:2], 1.0 / 1024, sm[:, 3:4], ALU.mult, ALU.subtract, r=[K("s2"), K("msq")],
           w=[K("var")])
    sc.ts("dve", sm[:, 6:7], sm[:, 4:5], eps, None, ALU.add, r=[K("var")], w=[K("ve")])
    sc.act(sm[:, 7:8], sm[:, 6:7], AF.Ln, r=[K("ve")], w=[K("lnv")])
    sc.act(sm[:, 5:6], sm[:, 7:8], AF.Exp, r=[K("lnv")], w=[K("rstd")], scale=-0.5)
    sc.ts("dve", xo, y, sm[:, 2:3], sm[:, 5:6], ALU.subtract, ALU.mult, r=list(keys_r) + [K("mean"), K("rstd")],
          w=[key_w])
    sc.tt("pool", xo, xo, gt, ALU.mult, r=[key_w] + gkeys, w=[key_w])
    sc.tt("pool", xo, xo, bt, ALU.add, r=[key_w] + gkeys, w=[key_w])


def phase_outproj(sc, nc, dr, l, xsrc):
    _PH[0] += 1
    with ExitStack() as ps:
        def sb(name, shape, dt):
            return ps.enter_context(nc.sbuf_tensor(f"u{_PH[0]}_p5_" + name, shape, dt))

        def pm(name, shape, dt):
            return ps.enter_context(nc.psum_tensor(f"u{_PH[0]}_p5_" + name, shape, dt))

        st = {}
        woa = sb("woa", [64, 8, 1024], BF16)
        wor = sb("wor", [128, 4, 1024], BF16)
        for h in range(8):
            sc.dma("pool", woa[:, h, :], dr["w_out"][l, h * 64:(h + 1) * 64, :], w=["woa"])
        for c in range(4):
            sc.dma("pool", wor[:, c, :], dr["w_out"][l, 512 + c * 128:512 + (c + 1) * 128, :], w=["wor"])
        gt = sb("gt", [128, 1024], F32)
        bt = sb("bt", [128, 1024], F32)
        sc.dma("sp", gt[:], dr["ln1_g"][l].partition_broadcast(128), w=["gt"])
        sc.dma("sp", bt[:], dr["ln1_b"][l].partition_broadcast(128), w=["bt"])
        attT = [sb(f"attT{i}", [64, 8, 512], BF16) for i in range(2)]
        lrT = [sb(f"lrT{i}", [128, 4, 512], BF16) for i in range(2)]
        xt = [sb(f"xt{i}", [128, 1024], F32) for i in range(2)]
        yy = [sb(f"yy{i}", [128, 1024], F32) for i in range(2)]
        xo = [sb(f"xo{i}", [128, 1024], F32) for i in range(2)]
        sm = [sb(f"sm{i}", [128, 8], F32) for i in range(2)]
        junk = [sb(f"junk{i}", [128, 1024], BF16) for i in range(2)]
        acc = [pm(f"acc{i}", [128, 512], F32) for i in range(4)]
        for J in range(NST):
            par = J % 2
            sc.dma("act", attT[par][:], dr["ATT_d"][:, :, J * 512:(J + 1) * 512].rearrange("h p n -> p h n"),
                   r=[("ATT_d", h, J) for h in range(8)], w=[("attT", par)])
            sc.dma("act", lrT[par][:], dr["LR_d"][:, :, J * 512:(J + 1) * 512].rearrange("c p n -> p c n"),
                   r=[("LR_d", c, J) for c in range(4)], w=[("lrT", par)])
            for t in range(4):
                i = 4 * J + t
                xi, x_ = rot(xt, st, "xt")
                sc.dma("act", x_[:], xsrc[i * 128:(i + 1) * 128, :], w=[("xt", xi)])
                yi, y = rot(yy, st, "yy")
                for n in range(2):
                    ai, a = rot(acc, st, "acc")
                    for h in range(8):
                        sc.mm(a[:], attT[par][:, h, t * 128:(t + 1) * 128], woa[:, h, n * 512:(n + 1) * 512],
                              start=(h == 0), stop=False, r=[("attT", par), "woa"], w=[("acc", ai)], sig=False)
                    for c in range(4):
                        sc.mm(a[:], lrT[par][:, c, t * 128:(t + 1) * 128], wor[:, c, n * 512:(n + 1) * 512],
                              start=False, stop=(c == 3), r=[("lrT", par), "wor"], w=[("acc", ai)], sig=(c == 3))
                    sc.stt("dve", y[:, n * 512:(n + 1) * 512], x_[:, n * 512:(n + 1) * 512], ALPHA, a[:], ALU.mult,
                           ALU.add, r=[("xt", xi), ("acc", ai)], w=[("yy", yi)])
                oi, o = rot(xo, st, "xo")
                layer_norm_tile(sc, y[:], o[:], gt[:], bt[:], sm[oi % 2], junk[oi % 2], 1e-5, [("yy", yi)], ("xo", oi),
                                ["gt", "bt"], tag=oi % 2)
                sc.dma("sp", dr["X1_d"][i * 128:(i + 1) * 128, :], o[:], r=[("xo", oi)], w=[("X1_d", i)])
    sc.barrier()


def phase_ffn(sc, nc, dr, l, experts, dff, xdst, router=None):
    _PH[0] += 1
    GC = 4
    nchunks = dff // 128
    groups = [(g, min(GC, nchunks - g)) for g in range(0, nchunks, GC)]
    HT = 2048
    with ExitStack() as ps:
        def sb(name, shape, dt):
            return ps.enter_context(nc.sbuf_tensor(f"u{_PH[0]}_p6_" + name, shape, dt))

        def pm(name, shape, dt):
            return ps.enter_context(nc.psum_tensor(f"u{_PH[0]}_p6_" + name, shape, dt))

        st = {}
        identb = sb("identb", [128, 128], BF16)
        sc.dma("sp", identb[:], dr["ident_bf"], w=["identb"])
        gt = sb("gt", [128, 1024], F32)
        bt = sb("bt", [128, 1024], F32)
        sc.dma("sp", gt[:], dr["ln2_g"][l].partition_broadcast(128), w=["gt"])
        sc.dma("sp", bt[:], dr["ln2_b"][l].partition_broadcast(128), w=["bt"])
        accs = sb("accs", [128, HT // 128, 1024], F32)
        xT = sb("xT", [128, 8, HT], BF16)
        wg = [sb(f"wg{i}", [128, 8, GC * 128], BF16) for i in range(2)]
        wu = [sb(f"wu{i}", [128, 8, GC * 128], BF16) for i in range(2)]
        wd = [sb(f"wd{i}", [128, GC, 1024], BF16) for i in range(2)]
        hT = [sb(f"hT{i}", [128, GC, 512], BF16) for i in range(2)]
        sg = [sb(f"sg{i}", [128, 512], F32) for i in range(2)]
        xt = [sb(f"xt{i}", [128, 1024], F32) for i in range(2)]
        xb = [sb(f"xb{i}", [128, 1024], BF16) for i in range(2)]
        yy = [sb(f"yy{i}", [128, 1024], F32) for i in range(2)]
        xo = [sb(f"xo{i}", [128, 1024], F32) for i in range(2)]
        sm = [sb(f"sm{i}", [128, 8], F32) for i in range(2)]
        junk = [sb(f"junk{i}", [128, 1024], BF16) for i in range(2)]
        pg = [pm(f"pg{i}", [128, 512], F32) for i in range(2)]
        pu = [pm(f"pu{i}", [128, 512], F32) for i in range(2)]
        po = [pm(f"po{i}", [128, 512], F32) for i in range(2)]
        pT = [pm(f"pT{i}", [128, 1024], BF16) for i in range(2)]
        if router is not None:
            identf = sb("identf", [128, 128], F32)
            sc.dma("sp", identf[:], dr["ident_f"], w=["identf"])
            wr = sb("wr", [128, 8, 8], F32)
            sc.dma("sp", wr[:], router.rearrange("(kc p) e -> p kc e", p=128), w=["wr"])
            xTf = sb("xTf", [128, 8, 128], F32)
            Gt = sb("Gt", [128, NT, 8], F32)
            lg = sb("lg", [128, 8], F32)
            r8 = sb("r8", [128, 8], F32)
            rs = sb("rs", [128, 4], F32)
            rtmp = sb("rtmp", [128, 2, 8], F32)
        ev = ["act", "dve"]
        for half in range(S_ // HT):
            for tt_ in range(HT // 128):
                i = half * (HT // 128) + tt_
                xi, x_ = rot(xt, st, "xt")
                sc.dma("act", x_[:], dr["X1_d"][i * 128:(i + 1) * 128, :], r=[("X1_d", i)], w=[("xt", xi)])
                bi, xb_ = rot(xb, st, "xb")
                sc.copy("pool", xb_[:], x_[:], r=[("xt", xi)], w=[("xb", bi)])
                pi, bank = rot(pT, st, "pT")
                for kc in range(8):
                    sc.tr(bank[:, kc * 128:(kc + 1) * 128], xb_[:, kc * 128:(kc + 1) * 128], identb[:],
                          r=[("xb", bi), "identb"], w=[("pT", pi)], sig=(kc == 7))
                sc.copy(ev[tt_ % 2], xT[:, :, tt_ * 128:(tt_ + 1) * 128], bank[:].rearrange("p (k t) -> p k t", t=128),
                        r=[("pT", pi)], w=[("xT", tt_ // 4)])
                if router is not None:
                    for q4 in range(2):
                        gi, gb_ = rot(pg, st, "pg")
                        for k4 in range(4):
                            kc = q4 * 4 + k4
                            sc.tr(gb_[:, k4 * 128:(k4 + 1) * 128], x_[:, kc * 128:(kc + 1) * 128], identf[:],
                                  r=[("xt", xi), "identf"], w=[("pg", gi)], sig=(k4 == 3))
                        sc.copy(ev[q4], xTf[:, q4 * 4:q4 * 4 + 4, :], gb_[:].rearrange("p (k t) -> p k t", t=128),
                                r=[("pg", gi)], w=[("xTf", q4)])
                    ui, ub = rot(pu, st, "pu")
                    for kc in range(8):
                        sc.mm(ub[:, 0:8], xTf[:, kc, :], wr[:, kc, :], start=(kc == 0), stop=(kc == 7),
                              r=[("xTf", kc // 4), "wr"], w=[("pu", ui)], sig=(kc == 7))
                    sc.copy("dve", lg[:], ub[:, 0:8], r=[("pu", ui)], w=["lg"])
                    sc.op("dve", lambda e: e.max(out=r8[:], in_=lg[:]), r=["lg"], w=["r8"])
                    sc.tt("dve", rs[:, 0:1], r8[:, 0:1], r8[:, 1:2], ALU.subtract, r=["r8"], w=["rs0"])
                    sc.act(rs[:, 1:2], rs[:, 0:1], AF.Sigmoid, r=["rs0"], w=["rs1"])
                    sc.ts("dve", rs[:, 2:3], rs[:, 1:2], -1.0, 1.0, ALU.mult, ALU.add, r=["rs1"], w=["rs2"])
                    sc.ts("dve", rtmp[:, 0, :], lg[:], r8[:, 0:1], rs[:, 1:2], ALU.is_equal, ALU.mult,
                          r=["lg", "r8", "rs1"], w=["rtmp0"])
                    sc.ts("dve", rtmp[:, 1, :], lg[:], r8[:, 1:2], rs[:, 2:3], ALU.is_equal, ALU.mult,
                          r=["lg", "r8", "rs2"], w=["rtmp1"])
                    sc.tt("dve", Gt[:, i, :], rtmp[:, 0, :], rtmp[:, 1, :], ALU.add, r=["rtmp0", "rtmp1"],
                          w=[("Gt", i)])
            first = True
            for e, (wga, wua, wda) in enumerate(experts):
                for (g0, nch) in groups:
                    wi, wg_ = rot(wg, st, "wg")
                    _, wu_ = rot(wu, st, "wu")
                    _, wd_ = rot(wd, st, "wd")
                    c0, c1 = g0 * 128, (g0 + nch) * 128
                    for kc in range(8):
                        sc.dma("pool", wg_[:, kc, 0:nch * 128], wga[kc * 128:(kc + 1) * 128, c0:c1], w=[("wg", wi)])
                        sc.dma("pool", wu_[:, kc, 0:nch * 128], wua[kc * 128:(kc + 1) * 128, c0:c1], w=[("wu", wi)])
                    for c in range(nch):
                        sc.dma("pool", wd_[:, c, :], wda[c0 + c * 128:c0 + (c + 1) * 128, :], w=[("wd", wi)])
                    for s in range(HT // 512):
                        hi, h_ = rot(hT, st, "hT")
                        for c in range(nch):
                            gi, g_ = rot(pg, st, "pg")
                            ui, u_ = rot(pu, st, "pu")
                            for kc in range(8):
                                sc.mm(g_[:], wg_[:, kc, c * 128:(c + 1) * 128], xT[:, kc, s * 512:(s + 1) * 512],
                                      start=(kc == 0), stop=(kc == 7), r=[("wg", wi), ("xT", s)], w=[("pg", gi)],
                                      sig=(kc == 7))
                            for kc in range(8):
                                sc.mm(u_[:], wu_[:, kc, c * 128:(c + 1) * 128], xT[:, kc, s * 512:(s + 1) * 512],
                                      start=(kc == 0), stop=(kc == 7), r=[("wu", wi), ("xT", s)], w=[("pu", ui)],
                                      sig=(kc == 7))
                            si, s_ = rot(sg, st, "sg")
                            sc.act(s_[:], g_[:], AF.Silu, r=[("pg", gi)], w=[("sg", si)])
                            sc.tt("dve", h_[:, c, :], s_[:], u_[:], ALU.mult, r=[("sg", si), ("pu", ui)],
                                  w=[("hT", hi, c)])
                        for t in range(4):
                            tl = s * 4 + t
                            gi_tile = half * (HT // 128) + tl
                            for n in range(2):
                                oi, o_ = rot(po, st, "po")
                                for c in range(nch):
                                    sc.mm(o_[:], h_[:, c, t * 128:(t + 1) * 128], wd_[:, c, n * 512:(n + 1) * 512],
                                          start=(c == 0), stop=(c == nch - 1), r=[("hT", hi, c), ("wd", wi)],
                                          w=[("po", oi)], sig=(c == nch - 1))
                                dst = accs[:, tl, n * 512:(n + 1) * 512]
                                ak = ("accs", tl, n)
                                if router is None:
                                    if first:
                                        sc.copy("dve", dst, o_[:], r=[("po", oi)], w=[ak])
                                    else:
                                        sc.tt("dve", dst, o_[:], dst, ALU.add, r=[("po", oi), ak], w=[ak])
                                else:
                                    gsc = Gt[:, gi_tile, e:e + 1]
                                    if first:
                                        sc.ts("dve", dst, o_[:], gsc, None, ALU.mult, r=[("po", oi), ("Gt", gi_tile)],
                                              w=[ak])
                                    else:
                                        sc.stt("dve", dst, o_[:], gsc, dst, ALU.mult, ALU.add,
                                               r=[("po", oi), ("Gt", gi_tile), ak], w=[ak])
                    first = False
            for tl in range(HT // 128):
                i = half * (HT // 128) + tl
                xi, x_ = rot(xt, st, "xt")
                sc.dma("act", x_[:], dr["X1_d"][i * 128:(i + 1) * 128, :], r=[("X1_d", i)], w=[("xt", xi)])
                yi, y = rot(yy, st, "yy")
                sc.stt("dve", y[:], x_[:], ALPHA, accs[:, tl, :], ALU.mult, ALU.add,
                       r=[("xt", xi), ("accs", tl, 0), ("accs", tl, 1)], w=[("yy", yi)])
                oi, o = rot(xo, st, "xo")
                layer_norm_tile(sc, y[:], o[:], gt[:], bt[:], sm[oi % 2], junk[oi % 2], 1e-5, [("yy", yi)], ("xo", oi),
                                ["gt", "bt"], tag=oi % 2)
                sc.dma("sp", xdst[i * 128:(i + 1) * 128, :], o[:], r=[("xo", oi)], w=[("xdst", i)])
    sc.barrier()


CAP = 1280
NSLOT = 8 * CAP


def ind_dma(sc, nc, out, out_off, in_, in_off, r=(), w=(), **kw):
    i = sc.dnext
    sc.dnext = (sc.dnext + 1) % sc.NDS
    if sc.dval[i] > 0:
        sc._wait("pool", ("d", i, sc.dval[i]))
    sc.deps("pool", r, w)
    inst = nc.gpsimd.indirect_dma_start(out=out, out_offset=out_off, in_=in_, in_offset=in_off, **kw)
    sc.dval[i] += 16
    inst.then_inc(sc.dsem[i], 16)
    sc.commit(("d", i, sc.dval[i]), r, w)


def phase_moe(sc, nc, dr, l, xdst):
    _PH[0] += 1
    GC = 4
    nchunks = 3584 // 128
    groups = [(g, min(GC, nchunks - g)) for g in range(0, nchunks, GC)]
    NS = CAP // 128
    stiles = [(0, 512), (512, 512), (1024, 256)]
    router = dr["moe_router"][0]
    with ExitStack() as ps:
        def sb(name, shape, dt=F32):
            return ps.enter_context(nc.sbuf_tensor(f"u{_PH[0]}_p7_" + name, shape, dt))

        def pm(name, shape, dt):
            return ps.enter_context(nc.psum_tensor(f"u{_PH[0]}_p7_" + name, shape, dt))

        st = {}
        rb_slot = nc.gpsimd.alloc_register("rb_slot")
        nc.gpsimd.reg_mov(rb_slot, NSLOT - 1)
        rb_tok = nc.gpsimd.alloc_register("rb_tok")
        nc.gpsimd.reg_mov(rb_tok, S_ - 1)
        identb = sb("identb", [128, 128], BF16)
        sc.dma("sp", identb[:], dr["ident_bf"], w=["identb"])
        identf = sb("identf", [128, 128])
        sc.dma("sp", identf[:], dr["ident_f"], w=["identf"])
        ltri = sb("ltri", [128, 128], BF16)
        sc.dma("sp", ltri[:], dr["ltri_bf"], w=["ltri"])
        onesb = sb("onesb", [128, 128], BF16)
        sc.memset("pool", onesb[:], 1.0, w=["onesb"])
        ecap = sb("ecap", [128, 2, 8])
        sc.dma("sp", ecap[:], dr["ecap"], w=["ecap"])
        gt = sb("gt", [128, 1024])
        bt = sb("bt", [128, 1024])
        sc.dma("sp", gt[:], dr["ln2_g"][l].partition_broadcast(128), w=["gt"])
        sc.dma("sp", bt[:], dr["ln2_b"][l].partition_broadcast(128), w=["bt"])
        wr = sb("wr", [128, 8, 8])
        sc.dma("sp", wr[:], router.rearrange("(kc p) e -> p kc e", p=128), w=["wr"])
        tokall = sb("tokall", [128, NT], I32)
        sc.op("pool", lambda e: e.iota(tokall[:], pattern=[[128, NT]], base=0, channel_multiplier=1), w=["tokall"])
        zer = sb("zer", [128, 1024])
        sc.memset("pool", zer[:], 0.0, w=["zer"])
        fill = sb("fill", [128, NSLOT // 128], I32)
        sc.memset("pool", fill[:], S_, w=["fill"])
        sc.dma("sp", dr["IDX_d"].rearrange("(p f) o -> p (f o)", p=128), fill[:], r=["fill"], w=["IDXfill"])
        sc.dma("sp", dr["GATE_d"].rearrange("(p f) o -> p (f o)", p=128), zer[:, 0:NSLOT // 128].bitcast(I32), r=["zer"],
               w=["GATEfill"])
        for i in range(NT):
            sc.dma("sp", dr["ACC_d"][i * 128:(i + 1) * 128, :], zer[:], r=["zer"], w=[("ACCz", i)])
        accs = sb("accs", [128, NS, 1024])
        xeT = [sb(f"xeT{i}", [128, 8, CAP], BF16) for i in range(2)]
        wg = [sb(f"wg{i}", [128, 8, GC * 128], BF16) for i in range(2)]
        wu = [sb(f"wu{i}", [128, 8, GC * 128], BF16) for i in range(2)]
        wd = [sb(f"wd{i}", [128, GC, 1024], BF16) for i in range(2)]
        hT = [sb(f"hT{i}", [128, GC, 512], BF16) for i in range(2)]
        sg = [sb(f"sg{i}", [128, 512]) for i in range(2)]
        xt = [sb(f"xt{i}", [128, 1024]) for i in range(2)]
        xb = [sb(f"xb{i}", [128, 1024], BF16) for i in range(2)]
        xg = [sb(f"xg{i}", [128, 1024], BF16) for i in range(2)]
        yy = [sb(f"yy{i}", [128, 1024]) for i in range(3)]
        xo = [sb(f"xo{i}", [128, 1024]) for i in range(2)]
        sm = [sb(f"sm{i}", [128, 8]) for i in range(2)]
        junk = [sb(f"junk{i}", [128, 1024], BF16) for i in range(2)]
        xTf_2 = [sb(f"xTf{i}", [128, 8, 128]) for i in range(2)]
        lg_2 = [sb(f"lg{i}", [128, 8]) for i in range(2)]
        r8_2 = [sb(f"r8{i}", [128, 8]) for i in range(2)]
        rs = sb("rs", [128, NT * 2])
        rsd_2 = [sb(f"rsd{i}", [128, 1]) for i in range(2)]
        mk_2 = [sb(f"mk{i}", [128, 3, 8]) for i in range(2)]
        mkb_2 = [sb(f"mkb{i}", [128, 8], BF16) for i in range(2)]
        off = sb("off", [128, 8])
        rank_2 = [sb(f"rank{i}", [128, 8]) for i in range(2)]
        ovf_2 = [sb(f"ovf{i}", [128, 8]) for i in range(2)]
        dtmp_2 = [sb(f"dtmp{i}", [128, 2, 8]) for i in range(2)]
        dst_f_2 = [sb(f"dst_f{i}", [128, 2]) for i in range(2)]
        dst_i = sb("dst_i", [128, NT * 2], I32)
        idxe = [sb(f"idxe{i}", [128, NS], I32) for i in range(2)]
        gate_e = [sb(f"gate_e{i}", [128, NS]) for i in range(2)]
        pg = [pm(f"pg{i}", [128, 512], F32) for i in range(2)]
        pu = [pm(f"pu{i}", [128, 512], F32) for i in range(2)]
        po = [pm(f"po{i}", [128, 512], F32) for i in range(2)]
        pT = [pm(f"pT{i}", [128, 1024], BF16) for i in range(2)]
        ev = ["act", "dve"]
        sc.copy("dve", off[:], ecap[:, 0, :], r=["ecap"], w=["off"])

        for i in range(NT):
            p2 = i % 2
            xTf, lg, r8, rsd, mk, mkb, rank, ovf, dtmp, dst_f = (xTf_2[p2], lg_2[p2], r8_2[p2], rsd_2[p2], mk_2[p2], mkb_2[p2],
                                                                  rank_2[p2], ovf_2[p2], dtmp_2[p2], dst_f_2[p2])
            xi, x_ = rot(xt, st, "xt")
            sc.dma("act", x_[:], dr["X1_d"][i * 128:(i + 1) * 128, :], r=[("X1_d", i)], w=[("xt", xi)])
            bi, xb_ = rot(xb, st, "xb")
            sc.copy("pool", xb_[:], x_[:], r=[("xt", xi)], w=[("xb", bi)])
            sc.dma("sp", dr["X1B_d"][i * 128:(i + 1) * 128, :], xb_[:], r=[("xb", bi)], w=[("X1B_d", i)])
            for q4 in range(2):
                gi, gb_ = rot(pg, st, "pg")
                for k4 in range(4):
                    kc = q4 * 4 + k4
                    sc.tr(gb_[:, k4 * 128:(k4 + 1) * 128], x_[:, kc * 128:(kc + 1) * 128], identf[:],
                          r=[("xt", xi), "identf"], w=[("pg", gi)], sig=(k4 == 3))
                sc.copy(ev[q4], xTf[:, q4 * 4:q4 * 4 + 4, :], gb_[:].rearrange("p (k t) -> p k t", t=128),
                        r=[("pg", gi)], w=[("xTf", p2, q4)])
            ui, ub = rot(pu, st, "pu")
            for kc in range(8):
                sc.mm(ub[:, 0:8], xTf[:, kc, :], wr[:, kc, :], start=(kc == 0), stop=(kc == 7),
                      r=[("xTf", p2, kc // 4), "wr"], w=[("pu", ui)], sig=(kc == 7))
            sc.copy("dve", lg[:], ub[:, 0:8], r=[("pu", ui)], w=[("lg", p2)])
            sc.op("dve", lambda e: e.max(out=r8[:], in_=lg[:]), r=[("lg", p2)], w=[("r8", p2)])
            sc.tt("dve", rsd[:], r8[:, 0:1], r8[:, 1:2], ALU.subtract, r=[("r8", p2)], w=[("rsd", p2)])
            sc.act(rs[:, 2 * i:2 * i + 1], rsd[:], AF.Sigmoid, r=[("rsd", p2)], w=[("rs", i)])
            sc.ts("dve", rs[:, 2 * i + 1:2 * i + 2], rs[:, 2 * i:2 * i + 1], -1.0, 1.0, ALU.mult, ALU.add, r=[("rs", i)], w=[("rs", i)])
            sc.ts("dve", mk[:, 0, :], lg[:], r8[:, 0:1], None, ALU.is_equal, r=[("lg", p2), ("r8", p2)], w=[("mk0", p2)])
            sc.ts("dve", mk[:, 1, :], lg[:], r8[:, 1:2], None, ALU.is_equal, r=[("lg", p2), ("r8", p2)], w=[("mk1", p2)])
            sc.tt("dve", mk[:, 2, :], mk[:, 0, :], mk[:, 1, :], ALU.add, r=[("mk0", p2), ("mk1", p2)], w=[("mk2", p2)])
            sc.copy("dve", mkb[:], mk[:, 2, :], r=[("mk2", p2)], w=[("mkb", p2)])
            oi, ob = rot(po, st, "po")
            sc.mm(ob[:, 0:8], ltri[:], mkb[:], r=["ltri", ("mkb", p2)], w=[("po", oi)])
            sc.mm(ob[:, 8:16], onesb[:], mkb[:], r=["onesb", ("mkb", p2)], w=[("po", oi)])
            sc.tt("dve", rank[:], ob[:, 0:8], off[:], ALU.add, r=[("po", oi), "off"], w=[("rank", p2)])
            sc.tt("dve", off[:], ob[:, 8:16], off[:], ALU.add, r=[("po", oi), "off", ("rank", p2)], w=["off"])
            sc.tt("dve", ovf[:], rank[:], ecap[:, 1, :], ALU.is_ge, r=[("rank", p2), "ecap"], w=[("ovf", p2)])
            sc.stt("dve", rank[:], ovf[:], 1.0e6, rank[:], ALU.mult, ALU.add, r=[("ovf", p2), ("rank", p2)], w=[("rank", p2)])
            for k in range(2):
                sc.tt("dve", dtmp[:, k, :], mk[:, k, :], rank[:], ALU.mult, r=[("rank", p2), (f"mk{k}", p2)], w=[(f"dtmp{k}", p2)])
                sc.op("dve", lambda e: e.tensor_reduce(out=dst_f[:, k:k + 1], in_=dtmp[:, k, :], axis=AX.X, op=ALU.add),
                      r=[(f"dtmp{k}", p2)], w=[(f"dstf{k}", p2)])
            sc.copy("dve", dst_i[:, 2 * i:2 * i + 2], dst_f[:], r=[("dstf0", p2), ("dstf1", p2)], w=[("dsti", i)])
            for k in range(2):
                offk = bass.IndirectOffsetOnAxis(ap=dst_i[:, 2 * i + k:2 * i + k + 1], axis=0)
                ind_dma(sc, nc, dr["IDX_d"][:, :], offk, tokall[:, i:i + 1], None, r=[("dsti", i), "tokall", "IDXfill"],
                        w=[("IDXs", i, k)], bounds_check=rb_slot, oob_is_err=False)
                offk2 = bass.IndirectOffsetOnAxis(ap=dst_i[:, 2 * i + k:2 * i + k + 1], axis=0)
                ind_dma(sc, nc, dr["GATE_d"][:, :], offk2, rs[:, 2 * i + k:2 * i + k + 1].bitcast(I32), None, r=[("dsti", i), ("rs", i), "GATEfill"],
                        w=[("GATEs", i, k)], bounds_check=rb_slot, oob_is_err=False)
        allidx = [("IDXs", i, k) for i in range(NT) for k in range(2)]
        allgate = [("GATEs", i, k) for i in range(NT) for k in range(2)]

        for i in range(2):
            sc.memset("pool", xg[i][:], 0.0, w=[("xg", i)])
        idx_of = {}

        def gather(e):
            ii, idx_ = rot(idxe, st, "idxe")
            _, gte = rot(gate_e, st, "gate_e")
            idx_of[e] = (ii, idx_, gte)
            sc.dma("sp", idx_[:], dr["IDX_d"][e * CAP:(e + 1) * CAP, :].rearrange("(s p) o -> p (s o)", p=128),
                   r=allidx + ["IDXfill"], w=[("idxe", ii)], allow_slow_non_contiguous=True)
            sc.dma("sp", gte[:].bitcast(I32),
                   dr["GATE_d"][e * CAP:(e + 1) * CAP, :].rearrange("(s p) o -> p (s o)", p=128),
                   r=allgate + ["GATEfill"], w=[("gate_e", ii)], allow_slow_non_contiguous=True)
            xe = xeT[e % 2]
            for s in range(NS):
                gi_, xg_ = rot(xg, st, "xg")
                ind_dma(sc, nc, xg_[:, :], None, dr["X1B_d"][:, :],
                        bass.IndirectOffsetOnAxis(ap=idx_[:, s:s + 1], axis=0),
                        r=[("idxe", ii), ("xg", gi_)] + [("X1B_d", i) for i in range(NT)], w=[("xg", gi_)],
                        bounds_check=rb_tok, oob_is_err=False)
                pi, bank = rot(pT, st, "pT")
                for kc in range(8):
                    sc.tr(bank[:, kc * 128:(kc + 1) * 128], xg_[:, kc * 128:(kc + 1) * 128], identb[:],
                          r=[("xg", gi_), "identb"], w=[("pT", pi)], sig=(kc == 7))
                sc.copy(ev[s % 2], xe[:, :, s * 128:(s + 1) * 128], bank[:].rearrange("p (k t) -> p k t", t=128),
                        r=[("pT", pi)], w=[("xeT", e % 2, s // 4)])

        wbuf = {}

        def load_w(k):
            e, (g0, nch) = work[k]
            wi, wg_ = rot(wg, st, "wg")
            _, wu_ = rot(wu, st, "wu")
            _, wd_ = rot(wd, st, "wd")
            wbuf[k] = (wi, wg_, wu_, wd_)
            wga, wua, wda = dr["moe_w_gate"][0, e], dr["moe_w_up"][0, e], dr["moe_w_down"][0, e]
            c0, c1 = g0 * 128, (g0 + nch) * 128
            for kc in range(8):
                sc.dma("pool", wg_[:, kc, 0:nch * 128], wga[kc * 128:(kc + 1) * 128, c0:c1], w=[("wg", wi)])
                sc.dma("pool", wu_[:, kc, 0:nch * 128], wua[kc * 128:(kc + 1) * 128, c0:c1], w=[("wu", wi)])
            for c in range(nch):
                sc.dma("pool", wd_[:, c, :], wda[c0 + c * 128:c0 + (c + 1) * 128, :], w=[("wd", wi)])

        work = [(e, grp) for e in range(8) for grp in groups]
        gather(0)
        load_w(0)
        for k, (e, (g0, nch)) in enumerate(work):
            if k + 1 < len(work):
                load_w(k + 1)
            gidx = groups.index((g0, nch))
            if gidx == 3 and e + 1 < 8:
                gather(e + 1)
            wi, wg_, wu_, wd_ = wbuf.pop(k)
            xe = xeT[e % 2]
            first = gidx == 0
            for sti, (s0, sn) in enumerate(stiles):
                hi, h_ = rot(hT, st, "hT")
                for c in range(nch):
                    gi, g_ = rot(pg, st, "pg")
                    ui, u_ = rot(pu, st, "pu")
                    for kc in range(8):
                        sc.mm(g_[:, 0:sn], wg_[:, kc, c * 128:(c + 1) * 128], xe[:, kc, s0:s0 + sn],
                              start=(kc == 0), stop=(kc == 7), r=[("wg", wi), ("xeT", e % 2, sti)], w=[("pg", gi)],
                              sig=(kc == 7))
                    for kc in range(8):
                        sc.mm(u_[:, 0:sn], wu_[:, kc, c * 128:(c + 1) * 128], xe[:, kc, s0:s0 + sn],
                              start=(kc == 0), stop=(kc == 7), r=[("wu", wi), ("xeT", e % 2, sti)], w=[("pu", ui)],
                              sig=(kc == 7))
                    si, s_ = rot(sg, st, "sg")
                    sc.act(s_[:, 0:sn], g_[:, 0:sn], AF.Silu, r=[("pg", gi)], w=[("sg", si)])
                    sc.tt("dve", h_[:, c, 0:sn], s_[:, 0:sn], u_[:, 0:sn], ALU.mult, r=[("sg", si), ("pu", ui)],
                          w=[("hT", hi, c)])
                for t in range(sn // 128):
                    tl = s0 // 128 + t
                    for n in range(2):
                        oi, o_ = rot(po, st, "po")
                        for c in range(nch):
                            sc.mm(o_[:], h_[:, c, t * 128:(t + 1) * 128], wd_[:, c, n * 512:(n + 1) * 512],
                                  start=(c == 0), stop=(c == nch - 1), r=[("hT", hi, c), ("wd", wi)],
                                  w=[("po", oi)], sig=(c == nch - 1))
                        dst = accs[:, tl, n * 512:(n + 1) * 512]
                        ak = ("accs", tl)
                        if first:
                            sc.copy("dve", dst, o_[:], r=[("po", oi)], w=[ak])
                        else:
                            sc.tt("dve", dst, o_[:], dst, ALU.add, r=[("po", oi), ak], w=[ak])
            if gidx == len(groups) - 1:
                ii, idx_, gte = idx_of[e]
                prev = [("ACCz", i) for i in range(NT)] if e == 0 else [("ACCs", e - 1, s) for s in range(NS)]
                for s in range(NS):
                    yi, y = rot(yy, st, "yy")
                    sc.ts("dve", y[:], accs[:, s, :], gte[:, s:s + 1], None, ALU.mult,
                          r=[("accs", s), ("gate_e", ii)], w=[("yy", yi)])
                    ind_dma(sc, nc, dr["ACC_d"][:, :], bass.IndirectOffsetOnAxis(ap=idx_[:, s:s + 1], axis=0),
                            y[:, :], None, r=[("yy", yi), ("idxe", ii)] + prev, w=[("ACCs", e, s)],
                            bounds_check=rb_tok, oob_is_err=False, compute_op=ALU.add)

        lastk = [("ACCs", 7, s) for s in range(NS)]
        for i in range(NT):
            xi, x_ = rot(xt, st, "xt")
            sc.dma("act", x_[:], dr["X1_d"][i * 128:(i + 1) * 128, :], r=[("X1_d", i)], w=[("xt", xi)])
            oi, o = rot(xo, st, "xo")
            sc.dma("act", o[:], dr["ACC_d"][i * 128:(i + 1) * 128, :], r=lastk, w=[("xo", oi)])
            yi, y = rot(yy, st, "yy")
            sc.stt("dve", y[:], x_[:], ALPHA, o[:], ALU.mult, ALU.add, r=[("xt", xi), ("xo", oi)], w=[("yy", yi)])
            layer_norm_tile(sc, y[:], o[:], gt[:], bt[:], sm[oi % 2], junk[oi % 2], 1e-5, [("yy", yi)], ("xo", oi),
                                ["gt", "bt"], tag=oi % 2)
            sc.dma("sp", xdst[i * 128:(i + 1) * 128, :], o[:], r=[("xo", oi)], w=[("xdst", i)])
    sc.barrier()


SCRATCH = {
    "QK_d": ([2, 8, 64, S_], BF16), "V_d": ([S_, 520], BF16), "FM_d": ([12, 128, S_], F32),
    "ATT_d": ([8, 64, S_], BF16), "LR_d": ([4, 128, S_], BF16), "X1_d": ([S_, D_], F32), "X2_d": ([S_, D_], F32),
    "X1B_d": ([S_, D_], BF16), "IDX_d": ([NSLOT, 1], I32), "GATE_d": ([NSLOT, 1], I32), "ACC_d": ([S_, D_], F32),
}


def build(dbg=(), stop_after=None, skip=()):
    nc = bass.Bass("TRN2", target_bir_lowering=False)
    dr = {}

    def din(name, shape, dt):
        dr[name] = nc.dram_tensor(name, list(shape), dt, kind="ExternalInput").ap()

    din("x", [S_, D_], F32)
    din("pos", [S_, 1], I32)
    for n, shp in WEIGHT_SPECS.items():
        din(n, shp, F32)
    for n, (shp, dt) in CONST_SPECS.items():
        din(n, shp, dt)
    for n, (shp, dt) in SCRATCH.items():
        kind = "ExternalOutput" if n in dbg else "Internal"
        dr[n] = nc.dram_tensor(n, list(shp), dt, kind=kind).ap()
    dr["out"] = nc.dram_tensor("out", [S_, D_], F32, kind="ExternalOutput").ap()

    with ExitStack() as es:
        sc = Sch(nc, es)

        def run():
            for l in range(2):
                xsrc = dr["x"] if l == 0 else dr["X2_d"]
                steps = [
                    ("inproj", lambda: phase_inproj(sc, nc, dr, l, xsrc)),
                    ("attn", lambda: phase_attn(sc, nc, dr, co=lambda sb, pm: lru_co(sc, nc, dr, l, sb, pm, npsum=1))),
                    ("rwkv", lambda: phase_rwkv(sc, nc, dr, l)),
                    ("outproj", lambda: phase_outproj(sc, nc, dr, l, xsrc)),
                ]
                if l == 0:
                    steps.append(("ffn", lambda: phase_ffn(
                        sc, nc, dr, 0, [(dr["ffn_w_gate"][0], dr["ffn_w_up"][0], dr["ffn_w_down"][0])], 2816,
                        dr["X2_d"])))
                else:
                    ex = [(dr["moe_w_gate"][0, e], dr["moe_w_up"][0, e], dr["moe_w_down"][0, e]) for e in range(8)]
                    if DENSE_MOE:
                        steps.append(("ffn", lambda: phase_ffn(sc, nc, dr, 1, ex, 3584, dr["out"],
                                                               router=dr["moe_router"][0])))
                    else:
                        steps.append(("ffn", lambda: phase_moe(sc, nc, dr, 1, dr["out"])))
                for name, fn in steps:
                    if (l, name) in skip or name in skip:
                        continue
                    fn()
                    if stop_after == (l, name):
                        return

        run()
        sc.barrier()
    return nc


_CACHE = {}


def kernel(**inputs):
    if "nc" not in _CACHE:
        _CACHE["nc"] = build()
        _CACHE["consts"] = make_consts()
    nc = _CACHE["nc"]
    consts = _CACHE["consts"]
    x = np.ascontiguousarray(inputs["x"], dtype=np.float32)
    pos = np.ascontiguousarray(inputs["positions"], dtype=np.int32)
    shared = {n: np.ascontiguousarray(inputs[n], dtype=np.float32) for n in WEIGHT_SPECS}
    shared.update(consts)
    in_maps = []
    for b in range(8):
        m = dict(shared)
        m["x"] = x[b]
        m["pos"] = pos[b].reshape(S_, 1)
        in_maps.append(m)
    res = run_bass_kernel_spmd(nc, in_maps, core_ids=list(range(8)))
    return np.stack([np.asarray(r["out"], dtype=np.float32) for r in res.results], axis=0)
```
